# Optimizing a Trainium2 kernel written in Bass

```python
import jax, jax.numpy as jnp
from jax import lax
import numpy as np

D_MODEL = 2048
BATCH = 8
SEQ = 2048
DEPTH = 1

D_MIX = D_MODEL
FOURIER_WIDTH = D_MIX // 2
N_FOURIER_GROUPS = 4
FOURIER_GROUP = FOURIER_WIDTH // N_FOURIER_GROUPS
POOL_WIDTH = D_MIX - FOURIER_WIDTH
POOL_WINDOWS = (2, 4, 8, 16)
N_POOL_GROUPS = len(POOL_WINDOWS)
POOL_GROUP = POOL_WIDTH // N_POOL_GROUPS
N_BRANCHES = 2
N_EXPERTS = 16
EXPERT_FF = 1408
CAPACITY_FACTOR = 2
EPS = 1e-6

kernel_name = "hybrid_fourier_pool_ec_moe_encoder"


def rmsnorm(x, g):
    xf = x.astype(jnp.float32)
    y = xf * lax.rsqrt(jnp.mean(xf * xf, axis=-1, keepdims=True) + EPS)
    return (y * g.astype(jnp.float32)).astype(x.dtype)


def fourier_mixer(p_f, w_mix):
    B, S, _ = p_f.shape
    z = p_f.reshape(B, S, N_FOURIER_GROUPS, FOURIER_GROUP).astype(jnp.float32)
    z = jnp.fft.fftn(z, axes=(1, 3), norm="ortho").real.astype(p_f.dtype)
    y = jnp.einsum("bsgc,gcd->bsgd", z, w_mix)
    return y.reshape(B, S, FOURIER_WIDTH)


def pool_mixer(p_p, w_mix, scale):
    B, S, _ = p_p.shape
    z = p_p.reshape(B, S, N_POOL_GROUPS, POOL_GROUP).astype(jnp.float32)
    csum = jnp.concatenate([jnp.zeros((B, 1, N_POOL_GROUPS, POOL_GROUP), jnp.float32),
                            jnp.cumsum(z, axis=1)], axis=1)
    half = jnp.array([w // 2 for w in POOL_WINDOWS], jnp.int32)
    pos = jnp.arange(S, dtype=jnp.int32)[:, None]
    lo = jnp.clip(pos - half[None, :], 0, S)
    hi = jnp.clip(pos + half[None, :], 0, S)
    g_idx = jnp.arange(N_POOL_GROUPS, dtype=jnp.int32)[None, :]
    win_sum = csum[:, hi, g_idx] - csum[:, lo, g_idx]
    count = (hi - lo).astype(jnp.float32)[None, :, :, None]
    pooled = (win_sum / count - z).astype(p_p.dtype)
    y = jnp.einsum("bsgc,gcd->bsgd", pooled, w_mix)
    return y.reshape(B, S, POOL_WIDTH) * scale


def mixer_block(u, w_in, w_fourier_mix, w_pool_mix, pool_scale, w_branch_f, w_branch_p,
                w_gate, b_gate, w_out):
    p = jnp.einsum("bsd,dm->bsm", u, w_in)
    y_f = jnp.einsum("bsm,md->bsd", fourier_mixer(p[..., :FOURIER_WIDTH], w_fourier_mix), w_branch_f)
    y_p = jnp.einsum("bsm,md->bsd", pool_mixer(p[..., FOURIER_WIDTH:], w_pool_mix, pool_scale), w_branch_p)
    gates = jax.nn.sigmoid((jnp.einsum("bsd,dk->bsk", u, w_gate) + b_gate).astype(jnp.float32)).astype(u.dtype)
    g_f, g_p = gates[..., :D_MODEL], gates[..., D_MODEL:]
    merged = g_f * y_f + g_p * y_p
    return jnp.einsum("bsd,de->bse", merged, w_out)


def expert_choice_moe(u, w_router, w_gate_e, w_up_e, w_down_e):
    B, S, D = u.shape
    cap = CAPACITY_FACTOR * S // N_EXPERTS
    logits = jnp.einsum("bsd,de->bse", u, w_router).astype(jnp.float32)
    affinity = jax.nn.softmax(logits, axis=-1)
    gate, idx = lax.top_k(jnp.swapaxes(affinity, 1, 2), cap)
    xs = jax.vmap(lambda ub, ib: ub[ib])(u, idx)
    h = jax.nn.silu(jnp.einsum("becd,edf->becf", xs, w_gate_e)) * jnp.einsum("becd,edf->becf", xs, w_up_e)
    y = jnp.einsum("becf,efd->becd", h, w_down_e) * gate[..., None].astype(u.dtype)
    combine = lambda ib, yb: jnp.zeros((S, D), yb.dtype).at[ib.reshape(-1)].add(yb.reshape(-1, D))
    return jax.vmap(combine)(idx, y)


def setup_inputs(seed: int = 0) -> dict:
    key = jax.random.key(seed)
    ks = jax.random.split(key, 20)
    f32 = jnp.float32
    nrm = lambda k, shape, fan_in: jax.random.normal(k, shape, f32) * (fan_in ** -0.5)
    L = DEPTH
    return {
        "x": jax.random.normal(ks[0], (BATCH, SEQ, D_MODEL), f32),
        "norm_mix_g": 1.0 + 0.02 * jax.random.normal(ks[1], (L, D_MODEL), f32),
        "w_in": nrm(ks[2], (L, D_MODEL, D_MIX), D_MODEL),
        "w_fourier_mix": nrm(ks[3], (L, N_FOURIER_GROUPS, FOURIER_GROUP, FOURIER_GROUP), FOURIER_GROUP),
        "w_pool_mix": nrm(ks[4], (L, N_POOL_GROUPS, POOL_GROUP, POOL_GROUP), POOL_GROUP),
        "pool_scale": 1.0 + 0.02 * jax.random.normal(ks[5], (L, POOL_WIDTH), f32),
        "w_branch_f": nrm(ks[6], (L, FOURIER_WIDTH, D_MODEL), FOURIER_WIDTH),
        "w_branch_p": nrm(ks[7], (L, POOL_WIDTH, D_MODEL), POOL_WIDTH),
        "w_gate": nrm(ks[8], (L, D_MODEL, N_BRANCHES * D_MODEL), D_MODEL),
        "b_gate": 0.02 * jax.random.normal(ks[9], (L, N_BRANCHES * D_MODEL), f32),
        "w_out": nrm(ks[10], (L, D_MODEL, D_MODEL), D_MODEL),
        "norm_moe_g": 1.0 + 0.02 * jax.random.normal(ks[11], (L, D_MODEL), f32),
        "w_router": nrm(ks[12], (L, D_MODEL, N_EXPERTS), D_MODEL),
        "w_expert_gate": nrm(ks[13], (L, N_EXPERTS, D_MODEL, EXPERT_FF), D_MODEL),
        "w_expert_up": nrm(ks[14], (L, N_EXPERTS, D_MODEL, EXPERT_FF), D_MODEL),
        "w_expert_down": nrm(ks[15], (L, N_EXPERTS, EXPERT_FF, D_MODEL), EXPERT_FF),
        "norm_final_g": 1.0 + 0.02 * jax.random.normal(ks[16], (D_MODEL,), f32),
    }


def reference(x, norm_mix_g, w_in, w_fourier_mix, w_pool_mix, pool_scale, w_branch_f, w_branch_p,
              w_gate, b_gate, w_out, norm_moe_g, w_router, w_expert_gate, w_expert_up,
              w_expert_down, norm_final_g):
    h = x
    for l in range(DEPTH):
        u = rmsnorm(h, norm_mix_g[l])
        h = h + mixer_block(u, w_in[l], w_fourier_mix[l], w_pool_mix[l], pool_scale[l],
                            w_branch_f[l], w_branch_p[l], w_gate[l], b_gate[l], w_out[l])
        v = rmsnorm(h, norm_moe_g[l])
        h = h + expert_choice_moe(v, w_router[l], w_expert_gate[l], w_expert_up[l], w_expert_down[l])
    return rmsnorm(h, norm_final_g)
```

```python
import math
from contextlib import ExitStack

import numpy as np
import ml_dtypes
import concourse.bass as bass
import concourse.mybir as mybir
from concourse.bass_utils import run_bass_kernel_spmd

F32 = mybir.dt.float32
BF16 = mybir.dt.bfloat16
I32 = mybir.dt.int32
U32 = mybir.dt.uint32
AF = mybir.ActivationFunctionType
ALU = mybir.AluOpType
AX = mybir.AxisListType

S = 2048
D = 2048
NE = 16
FF = 1408
NFT = 11
CAP = 256
EPS = 1e-6
ENGS = ("pe", "act", "dve", "pool", "sp")


class Tok:
    __slots__ = ("sem", "val")

    def __init__(self, sem, val):
        self.sem = sem
        self.val = val


class DSem:
    def __init__(self, sem):
        self.sem = sem
        self.n = 0


class Builder:
    def __init__(self, nc, es):
        self.nc = nc
        self.es = es
        self.prog = {e: [] for e in ENGS}
        self.cnt = {e: 0 for e in ENGS}
        self.waited = {}
        self.done = {e: es.enter_context(nc.semaphore("done_" + e)) for e in ENGS}
        self.nsem = 0

    def dsem(self, name=None):
        self.nsem += 1
        return DSem(self.es.enter_context(self.nc.semaphore(name or ("ds%d" % self.nsem))))

    def wait(self, eng, toks):
        for t in toks:
            if t is None:
                continue
            if isinstance(t, (list, tuple)):
                self.wait(eng, t)
                continue
            key = (eng, id(t.sem))
            if self.waited.get(key, 0) >= t.val:
                continue
            self.waited[key] = t.val
            self.prog[eng].append(lambda e, t=t: e.wait_ge(t.sem, t.val))

    def op(self, eng, fn, waits=()):
        self.wait(eng, waits)
        self.cnt[eng] += 1
        n = self.cnt[eng]
        s = self.done[eng]
        self.prog[eng].append(lambda e: fn(e).then_inc(s, 1))
        return Tok(s, n)

    def op0(self, eng, fn, waits=()):
        self.wait(eng, waits)
        self.prog[eng].append(lambda e: fn(e))

    def dma(self, eng, out, in_, ds, waits=(), **kw):
        self.wait(eng, waits)
        ds.n += 16
        v = ds.n
        self.prog[eng].append(lambda e: e.dma_start(out=out, in_=in_, **kw).then_inc(ds.sem, 16))
        return Tok(ds.sem, v)

    def raw(self, eng, fn, ds, waits=()):
        self.wait(eng, waits)
        ds.n += 16
        v = ds.n
        self.prog[eng].append(lambda e: fn(e).then_inc(ds.sem, 16))
        return Tok(ds.sem, v)


class Stream:
    def __init__(self, B, slots, chunks, start_waits=(), after_issue=None, extra_waits=None, hwdge_from=None):
        self.hwdge_from = hwdge_from
        self.after_issue = after_issue
        self.extra_waits = extra_waits or {}
        self.B = B
        self.slots = slots
        self.ns = len(slots)
        self.chunks = chunks
        self.sems = [B.dsem() for _ in slots]
        self.issued = 0
        self.cur = 0
        self.rel = []
        self.toks = []
        self.start_waits = list(start_waits)

    def _issue(self, j):
        src, nel = self.chunks[j]
        sl = j % self.ns
        waits = list(self.start_waits) if j < self.ns else [self.rel[j - self.ns]]
        dst = self.slots[sl][:, 0:nel]
        waits = waits + list(self.extra_waits.get(j, []))
        eng = "pool"
        if self.hwdge_from is not None and j in self.hwdge_from:
            eng = "sp"
        self.toks.append(self.B.dma(eng, dst, src, self.sems[sl], waits))
        if self.after_issue is not None:
            self.after_issue(j)

    def get(self):
        hi = min(self.cur + self.ns - 1, len(self.chunks) - 1)
        while self.issued <= hi and (self.issued < self.ns or self.issued - self.ns < len(self.rel)):
            self._issue(self.issued)
            self.issued += 1
        j = self.cur
        assert j < self.issued, "stream chunk not issued (ring deadlock)"
        self.cur += 1
        nel = self.chunks[j][1]
        return self.slots[j % self.ns][:, 0:nel], self.toks[j]

    def release(self, tok):
        self.rel.append(tok)

    def prefetch(self):
        while self.issued < min(self.ns, len(self.chunks)):
            self._issue(self.issued)
            self.issued += 1


def build_nc(stage=99, debug=False):
    nc = bass.Bass("TRN2", target_bir_lowering=False)

    def din(name, shape, dt=F32):
        return nc.dram_tensor(name, shape, dt, kind="ExternalInput").ap()

    x = din("x", [S, D])
    win = din("win", [4, 128, 8192])
    wmix = din("wmix", [16, 128, 6144])
    wout = din("wout", [4, 128, 8192])
    wfm = din("wfm", [128, 2048])
    wpm = din("wpm", [128, 2048])
    egu = din("egu", [NE, NFT, 128, 4096])
    edn = din("edn", [NE, 8, 128, 2816])
    dft = din("dft", [2, 2, 128, 8192], BF16)
    ccsc = din("ccsc", [128, 1024], BF16)
    poolr = din("poolr", [4, 2, 128, 6144], BF16)
    gmix_d = din("gmix", [128, 16])
    bgate_d = din("bgate", [128, 32])
    pscale_d = din("pscale", [128, 8])
    gmoe_d = din("gmoe", [128, D])
    gfin_d = din("gfin", [128, D])
    wr_d = din("wr", [128, 256])
    identb_d = din("identb", [128, 128], BF16)
    alt_d = din("alt", [128, 16], BF16)
    identf_d = din("identf", [128, 128])
    out = nc.dram_tensor("out", [S, D], F32, kind="ExternalOutput").ap()
    dk = "ExternalOutput" if debug else "Internal"
    uTd = nc.dram_tensor("uTd", [128, 16 * S], BF16, kind=dk).ap()
    hbuf = nc.dram_tensor("hbuf", [S, D], F32, kind=dk).ap()
    vbuf = nc.dram_tensor("vbuf", [S, D], BF16, kind=dk).ap()
    if debug:
        dbg = nc.dram_tensor("dbg", [128, 8192], F32, kind="ExternalOutput").ap()
    NCONV = 4
    NPART = 10
    E_PART = NE - NCONV - 1
    egu_bf = nc.dram_tensor("egu_bf", [NCONV + 1, NFT, 128, 4096], BF16, kind="Internal").ap()
    edn_bf = nc.dram_tensor("edn_bf", [NCONV, 8, 128, 2816], BF16, kind="Internal").ap()
    wout_bf = nc.dram_tensor("wout_bf", [4, 128, 8192], BF16, kind="Internal").ap()

    es = ExitStack()
    with es:
        B = Builder(nc, es)
        sb = lambda name, shape, dt: es.enter_context(nc.sbuf_tensor(name, shape, dt))
        K = 512
        arena = sb("arena", [128, 192 * K], BF16)
        rB = (0, 64 * K)
        rA = (64 * K, 128 * K)
        rC = (128 * K, 160 * K)
        rD = (160 * K, 192 * K)

        def bfv(lo, n):
            return arena[:, lo:lo + n]

        def f32v(lo, n):
            return arena[:, lo:lo + 2 * n].bitcast(F32)

        Gc = sb("Gc", [128, 4096], BF16)
        ccsc_s = sb("ccsc_s", [128, 1024], BF16)
        identb = sb("identb_s", [128, 128], BF16)
        identf = sb("identf_s", [128, 128], F32)
        gmix = sb("gmix_s", [128, 16], F32)
        bgate = sb("bgate_s", [128, 32], F32)
        pscale = sb("pscale_s", [128, 8], F32)
        wr_s = sb("wr_s", [128, 256], BF16)
        alt_s = sb("alt_s", [128, 16], BF16)
        y1024 = sb("y1024", [128, 8], BF16)
        stat = sb("stat", [128, 128], F32)
        afftm = sb("afftm", [128, 256], F32)
        idxT = sb("idxT", [128, 32], I32)
        gateT = sb("gateT", [128, 32], F32)
        ps = [es.enter_context(nc.psum_tensor("ps%d" % i, [128, 512], F32)) for i in range(8)]
        psfree = [None] * 8

        def psb(i):
            return ps[i][:, :].bitcast(BF16)

        cs = B.dsem("cs")
        ctoks = []
        for dst, src in ((identb, identb_d), (identf, identf_d), (gmix, gmix_d), (bgate, bgate_d),
                         (pscale, pscale_d), (ccsc_s, ccsc), (alt_s, alt_d)):
            ctoks.append(B.dma("sp", dst[:, :], src, cs))
        ctok = ctoks[-1]
        wrs = B.dsem("wrs")
        wr_tok = B.dma("pool", wr_s[:, :], wr_d, wrs)

        r1_slots = [bfv(rD[0], 16 * K), bfv(rD[0] + 16 * K, 16 * K)]
        ch1 = [(wfm, 2048)]
        for nb in range(4):
            ch1.append((win[nb], 8192))
        for kb in range(2):
            for c_s in range(2):
                ch1.append((dft[kb, c_s], 8192))
        for ib in range(4):
            for gp in range(2):
                ch1.append((poolr[ib, gp], 6144))
        for hf in range(2):
            for j in range(16):
                ch1.append((wmix[j], 6144))
        cvw = B.dsem("cvw")
        cvs = B.dsem("cvs")
        conv_pieces = []
        for ci in range(NCONV):
            e_ = NE - NCONV + ci
            for ft in range(NFT):
                conv_pieces.append((egu_bf[ci, ft], egu[e_, ft], cvs))
            for nq in range(8):
                conv_pieces.append((edn_bf[ci, nq], edn[e_, nq], cvs))
        for ft in range(NPART):
            conv_pieces.append((egu_bf[NCONV, ft], egu[E_PART, ft], cvs))
        conv_state = {"i": 0}

        def conv_emit(n):
            while n > 0 and conv_state["i"] < len(conv_pieces):
                dst, src, sm = conv_pieces[conv_state["i"]]
                B.dma("pool", dst, src, sm)
                conv_state["i"] += 1
                n -= 1

        conv_after = {}
        conv_after[2] = 10
        conv_after[4] = 5
        conv_after[5] = 4
        for j_ in range(6, 9):
            conv_after[j_] = 3
        for j_ in range(17, 49):
            conv_after[j_] = 1
        R1 = Stream(B, r1_slots, ch1, after_issue=lambda j_: conv_emit(conv_after.get(j_, 0)))

        wfm_v, wfm_tok = R1.get()
        wfm_v = wfm_v.rearrange("p (g c d) -> p g c d", g=4, c=2)
        ccv = ccsc_s[:, :].rearrange("p (s c d) -> p s c d", s=2, c=2)
        Gv = Gc[:, :].rearrange("p (i d) -> p i d", d=256)
        nrm = 1.0 / math.sqrt(S * 256.0)
        gi = 0
        last = None
        for g in range(4):
            for c_s in range(2):
                for cc in range(2):
                    bk = 4 + gi % 4
                    for kc in range(2):
                        f = lambda e, bk=bk, c_s=c_s, kc=kc, cc=cc, g=g: e.matmul(
                            ps[bk][:, 0:256], ccv[:, c_s, kc, cc * 128:(cc + 1) * 128], wfm_v[:, g, kc, :],
                            start=(kc == 0), stop=(kc == 1))
                        if kc == 0:
                            B.op0("pe", f, [wfm_tok, ctok, psfree[bk]])
                        else:
                            mt = B.op("pe", f)
                    sc = nrm if c_s == 0 else -nrm
                    idx = g * 4 + c_s * 2 + cc
                    last = B.op("act", lambda e, bk=bk, idx=idx, sc=sc: e.activation(
                        Gv[:, idx, :], ps[bk][:, 0:256], AF.Copy, scale=sc), [mt])
                    psfree[bk] = last
                    gi += 1
        R1.release(mt)
        G_tok = last

        uT = bfv(rA[0], 64 * K).rearrange("p (k t) -> p k t", k=16)
        xb = [f32v(rC[0], 2048), f32v(rC[0] + 8 * K, 2048)]
        xn = [bfv(rC[0] + 16 * K, 2048), bfv(rC[0] + 20 * K, 2048)]
        xs_sem = [B.dsem(), B.dsem()]
        ssq = stat[:, 0:16]
        rstd = stat[:, 16:32]
        cp_tok = [None, None]
        tr_tok = [None, None]
        uT_toks = []
        uts = B.dsem("uts")
        ut_store = []
        uTd_v = uTd.rearrange("p (k t) -> p k t", k=16)
        pf = bfv(rB[0], 32 * K).rearrange("p (t c) -> p t c", t=16)
        pp = bfv(rB[0] + 32 * K, 32 * K).rearrange("p (t c) -> p t c", t=16)
        pst = {"evn": 0, "mt": None}
        p_last = {}

        def p_group(nb, t, wv, wt, eng=None):
            dstp = pf if nb < 2 else pp
            c0 = (nb % 2) * 512
            bk = 4 + pst["evn"] % 4
            for k in range(16):
                f = lambda e, bk=bk, k=k, t=t, wv=wv: e.matmul(
                    ps[bk][:, :], uT[:, k, t * 128:(t + 1) * 128], wv[:, k, :], start=(k == 0), stop=(k == 15))
                if k < 15:
                    B.op0("pe", f, [wt, uT_toks[t], psfree[bk]])
                else:
                    mt = B.op("pe", f)
            if eng is None:
                eng = "act" if pst["evn"] % 2 == 0 else "dve"
            if eng == "act":
                ev = B.op("act", lambda e, bk=bk, t=t, dstp=dstp, c0=c0: e.activation(
                    dstp[:, t, c0:c0 + 512], ps[bk][:, :], AF.Copy), [mt])
            else:
                ev = B.op("dve", lambda e, bk=bk, t=t, dstp=dstp, c0=c0: e.tensor_copy(
                    dstp[:, t, c0:c0 + 512], ps[bk][:, :]), [mt])
            psfree[bk] = ev
            p_last[eng] = ev
            pst["evn"] += 1
            pst["mt"] = mt

        w0v, w0t = R1.get()
        w0v = w0v.rearrange("p (k n) -> p k n", k=16)
        w1v, w1t = R1.get()
        w1v = w1v.rearrange("p (k n) -> p k n", k=16)
        xts = {}

        def x_load(t):
            b = t % 2
            xts[t] = B.dma("sp", xb[b], x[t * 128:(t + 1) * 128, :], xs_sem[b], [cp_tok[b]])

        cps = {}

        def norm_chain(t):
            b = t % 2
            xt = xts[t]
            sq = B.op("act", lambda e, b=b, t=t: e.activation(xn[b], xb[b], AF.Square, accum_out=ssq[:, t:t + 1]),
                      [xt, tr_tok[b]])
            r1 = B.op("act", lambda e, t=t: e.activation(rstd[:, t:t + 1], ssq[:, t:t + 1], AF.Sqrt, bias=EPS,
                                                        scale=1.0 / D), [sq])
            r2 = B.op("dve", lambda e, t=t: e.reciprocal(rstd[:, t:t + 1], rstd[:, t:t + 1]), [r1])
            cp = B.op("act", lambda e, b=b, t=t: e.activation(xn[b], xb[b], AF.Copy, scale=rstd[:, t:t + 1]),
                      [r2, sq])
            cp_tok[b] = cp
            cps[t] = cp
            if t + 2 < 16:
                x_load(t + 2)

        x_load(0)
        x_load(1)
        norm_chain(0)
        for t in range(16):
            b = t % 2
            if t + 1 < 16:
                norm_chain(t + 1)
            cp = cps[t]
            bk0 = 0 if b == 0 else 2
            for k in range(16):
                bk = bk0 + k // 8
                f = lambda e, bk=bk, k=k, b=b: e.transpose(
                    psb(bk)[:, (k % 8) * 128:(k % 8 + 1) * 128], xn[b][:, k * 128:(k + 1) * 128], identb[:, :])
                if k < 15:
                    B.op0("pe", f, [cp, ctok, psfree[bk0], psfree[bk0 + 1]])
                else:
                    tr = B.op("pe", f)
            tr_tok[b] = tr
            for hh in range(2):
                bk = bk0 + hh
                ev = B.op("dve", lambda e, bk=bk, hh=hh, t=t: e.tensor_tensor(
                    uT[:, hh * 8:(hh + 1) * 8, t * 128:(t + 1) * 128],
                    psb(bk).rearrange("p (k c) -> p k c", k=8),
                    gmix[:, hh * 8:(hh + 1) * 8].unsqueeze(2).to_broadcast([128, 8, 128]),
                    ALU.mult), [tr, ctok])
                psfree[bk] = ev
            uT_toks.append(ev)
            if t % 4 == 3:
                tg = t // 4
                ut_store.append(B.dma("sp", uTd_v[:, :, tg * 512:(tg + 1) * 512],
                                      uT[:, :, tg * 512:(tg + 1) * 512], uts, [ev]))
            if t >= 1:
                p_group(0, t - 1, w0v, w0t, "dve")
                p_group(1, t - 1, w1v, w1t, "dve")
        s1a_done = [cp_tok[0], cp_tok[1], tr_tok[0], tr_tok[1]]
        p_group(0, 15, w0v, w0t)
        R1.release(pst["mt"])
        p_group(1, 15, w1v, w1t)
        R1.release(pst["mt"])
        for nb in range(2, 4):
            wv, wt = R1.get()
            wv = wv.rearrange("p (k n) -> p k n", k=16)
            for t in range(16):
                p_group(nb, t, wv, wt)
            R1.release(pst["mt"])
        s1b_mm = pst["mt"]
        p_toks = [p_last["act"], p_last["dve"]]

        YT = [bfv(rA[0], 16 * K).rearrange("p (m s k) -> p m s k", m=8, s=2),
              bfv(rA[0] + 16 * K, 16 * K).rearrange("p (m s k) -> p m s k", m=8, s=2)]
        zfmT = bfv(rA[0] + 32 * K, 32 * K).rearrange("p (m t) -> p m t", m=8)
        yt_read = [None, None]
        a_dead = [s1b_mm, ut_store[-1]]
        evn = 0
        zlast = {}
        tmpA = [f32v(rC[0] + 8 * K, 512), f32v(rC[0] + 10 * K, 512)]
        tmpA_rd = [None, None]
        arena_t = arena[:, :].tensor
        pstep = arena[:, :].ap[0][0]

        def rev_cols(m, hi_col, n):
            base = zfmT[:, m, hi_col:hi_col + 1]
            return bass.AP(tensor=arena_t, offset=base.offset, ap=[[pstep, 128], [-1, n]])

        zi = 0
        for kb in range(2):
            yb = kb % 2
            ylast = {}
            for c_s in range(2):
                dv, dt_ = R1.get()
                dv = dv.rearrange("p (c k) -> p c k", c=16)
                for m in range(8):
                    bk = evn % 4
                    for c in range(16):
                        f = lambda e, bk=bk, c=c, m=m, dv=dv: e.matmul(
                            ps[bk][:, :], pf[:, c, m * 128:(m + 1) * 128], dv[:, c, :], start=(c == 0), stop=(c == 15))
                        if c < 15:
                            B.op0("pe", f, [dt_, p_toks, psfree[bk]])
                        else:
                            mt = B.op("pe", f)
                    eng = "act" if evn % 2 == 0 else "dve"
                    w = [mt, yt_read[yb]] + a_dead
                    if eng == "act":
                        ev = B.op("act", lambda e, bk=bk, m=m, c_s=c_s, yb=yb: e.activation(
                            YT[yb][:, m, c_s, :], ps[bk][:, :], AF.Copy), w)
                    else:
                        ev = B.op("dve", lambda e, bk=bk, m=m, c_s=c_s, yb=yb: e.tensor_copy(
                            YT[yb][:, m, c_s, :], ps[bk][:, :]), w)
                    psfree[bk] = ev
                    ylast[eng] = ev
                    evn += 1
                R1.release(mt)
            yw = [ylast["act"], ylast["dve"], G_tok]
            for g in range(4):
                for dtl in range(2):
                    m = g * 2 + dtl
                    ba = 4 + 2 * (zi % 2)
                    bb = ba + 1
                    tb_ = zi % 2
                    mts = []
                    for c_s in range(2):
                        bk = ba if c_s == 0 else bb
                        for cc in range(2):
                            f = lambda e, bk=bk, g=g, c_s=c_s, cc=cc, dtl=dtl, yb=yb: e.matmul(
                                ps[bk][:, :], Gv[:, g * 4 + c_s * 2 + cc, dtl * 128:(dtl + 1) * 128],
                                YT[yb][:, g * 2 + cc, c_s, :], start=(cc == 0), stop=(cc == 1))
                            if cc == 0:
                                B.op0("pe", f, yw + [psfree[bk]])
                            else:
                                mts.append(B.op("pe", f))
                    mt = mts[1]
                    ca = B.op("act", lambda e, ba=ba, tb_=tb_: e.activation(tmpA[tb_], ps[ba][:, :], AF.Copy),
                              [mts[0], tmpA_rd[tb_], s1a_done])
                    d1 = B.op("dve", lambda e, bb=bb, tb_=tb_, m=m, kb=kb: e.tensor_tensor(
                        zfmT[:, m, kb * 512:(kb + 1) * 512], tmpA[tb_], ps[bb][:, :], ALU.add),
                        [ca, mts[1]] + a_dead)
                    if kb == 0:
                        d2 = B.op("dve", lambda e, bb=bb, tb_=tb_, m=m: e.tensor_tensor(
                            rev_cols(m, 2047, 511), tmpA[tb_][:, 1:512], ps[bb][:, 1:512], ALU.subtract), [d1])
                    else:
                        d2 = B.op("dve", lambda e, bb=bb, tb_=tb_, m=m: e.tensor_tensor(
                            rev_cols(m, 1536, 512), tmpA[tb_][:, 0:512], ps[bb][:, 0:512], ALU.subtract), [d1])
                    psfree[ba] = ca
                    psfree[bb] = d2
                    tmpA_rd[tb_] = d2
                    zlast["dve"] = d2
                    zi += 1
            yt_read[yb] = mt
        for m in range(8):
            for c in range(16):
                f = lambda e, c=c, m=m: e.matmul(ps[0][:, m:m + 1], pf[:, c, m * 128:(m + 1) * 128],
                                                 alt_s[:, c:c + 1], start=(c == 0), stop=(c == 15))
                if c < 15 or m < 7:
                    B.op0("pe", f, [p_toks, psfree[0], ctok])
                else:
                    mt = B.op("pe", f)
        yc = B.op("act", lambda e: e.activation(y1024[:, :], ps[0][:, 0:8], AF.Copy), [mt])
        psfree[0] = yc
        for g in range(4):
            for dtl in range(2):
                m = g * 2 + dtl
                for cc in range(2):
                    f = lambda e, g=g, cc=cc, dtl=dtl, m=m: e.matmul(
                        ps[1][:, m:m + 1], Gv[:, g * 4 + cc, dtl * 128:(dtl + 1) * 128],
                        y1024[:, g * 2 + cc:g * 2 + cc + 1], start=(cc == 0), stop=(cc == 1))
                    if m < 7 or cc == 0:
                        B.op0("pe", f, [yc, psfree[1], G_tok])
                    else:
                        mt = B.op("pe", f)
        zc = B.op("act", lambda e: e.activation(zfmT[:, :, 1024], ps[1][:, 0:8], AF.Copy), [mt] + a_dead)
        psfree[1] = zc
        zlast["act"] = zc
        s2a_mm = mt
        zfm_toks = [zlast["act"], zlast["dve"]]

        wpm_s = bfv(rC[0], 2048).rearrange("p (g c d) -> p g c d", g=4, c=2)
        wpms = B.dsem("wpms")
        wpm_tok = B.dma("pool", bfv(rC[0], 2048), wpm, wpms, s1a_done)
        pooledT = [bfv(rB[0], 4 * K * 2).rearrange("p (m k) -> p m k", m=8),
                   bfv(rB[0] + 8 * K, 4 * K * 2).rearrange("p (m k) -> p m k", m=8)]
        zpmT = bfv(rA[0], 32 * K).rearrange("p (m t) -> p m t", m=8)
        pl_read = [None, None]
        evn = 0
        zplast = {}
        for ib in range(4):
            pb = ib % 2
            pl = {}
            for gp in range(2):
                rv, rt = R1.get()
                rv = rv.rearrange("p (g j k) -> p g j k", g=2, j=6)
                for gl in range(2):
                    g = gp * 2 + gl
                    for ct in range(2):
                        bk = evn % 4
                        jjs = [jj for jj in range(6) if 0 <= 4 * ib - 1 + jj < 16]
                        for n, jj in enumerate(jjs):
                            jt = 4 * ib - 1 + jj
                            f = lambda e, bk=bk, jt=jt, g=g, ct=ct, rv=rv, gl=gl, jj=jj, n=n, L=len(jjs): e.matmul(
                                ps[bk][:, :], pp[:, jt, g * 256 + ct * 128:g * 256 + (ct + 1) * 128],
                                rv[:, gl, jj, :], start=(n == 0), stop=(n == L - 1))
                            if n < len(jjs) - 1:
                                B.op0("pe", f, [rt, p_toks, psfree[bk]])
                            else:
                                mt = B.op("pe", f)
                        eng = "act" if evn % 2 == 0 else "dve"
                        w = [mt, pl_read[pb], s2a_mm]
                        if eng == "act":
                            ev = B.op("act", lambda e, bk=bk, g=g, ct=ct, pb=pb: e.activation(
                                pooledT[pb][:, g * 2 + ct, :], ps[bk][:, :], AF.Copy), w)
                        else:
                            ev = B.op("dve", lambda e, bk=bk, g=g, ct=ct, pb=pb: e.tensor_copy(
                                pooledT[pb][:, g * 2 + ct, :], ps[bk][:, :]), w)
                        psfree[bk] = ev
                        pl[eng] = ev
                        evn += 1
                R1.release(mt)
            plw = [pl["act"], pl["dve"], wpm_tok]
            for g in range(4):
                for dtl in range(2):
                    bk = 4 + (g * 2 + dtl) % 4
                    for cc in range(2):
                        f = lambda e, bk=bk, g=g, cc=cc, dtl=dtl, pb=pb: e.matmul(
                            ps[bk][:, :], wpm_s[:, g, cc, dtl * 128:(dtl + 1) * 128],
                            pooledT[pb][:, g * 2 + cc, :], start=(cc == 0), stop=(cc == 1))
                        if cc == 0:
                            B.op0("pe", f, plw + [psfree[bk]])
                        else:
                            mt = B.op("pe", f)
                    m = g * 2 + dtl
                    eng = "act" if m % 2 == 0 else "dve"
                    if eng == "act":
                        ev = B.op("act", lambda e, bk=bk, m=m, ib=ib: e.activation(
                            zpmT[:, m, ib * 512:(ib + 1) * 512], ps[bk][:, :], AF.Copy, scale=pscale[:, m:m + 1]),
                            [mt, s2a_mm, ctok])
                    else:
                        ev = B.op("dve", lambda e, bk=bk, m=m, ib=ib: e.tensor_scalar(
                            zpmT[:, m, ib * 512:(ib + 1) * 512], ps[bk][:, :], pscale[:, m:m + 1], None, ALU.mult),
                            [mt, s2a_mm, ctok])
                    psfree[bk] = ev
                    zplast[eng] = ev
            pl_read[pb] = mt
        s2b_mm = mt
        z_toks = zfm_toks + [zplast["act"], zplast["dve"]]

        uTh = bfv(rB[0], 32 * K).rearrange("p (k t) -> p k t", k=16)
        mg = [bfv(rB[0] + 32 * K, 32 * K).rearrange("p (j t) -> p j t", j=16),
              bfv(rC[0], 32 * K).rearrange("p (j t) -> p j t", j=16)]
        tmpf = Gc[:, :].bitcast(F32).rearrange("p (i k) -> p i k", k=512)
        uth_s = B.dsem("uth")
        uth_s2 = B.dsem("uth2")
        uTd_v = uTd.rearrange("p (k t) -> p k t", k=16)
        s3b_mm = None
        mg_toks = []
        tmp_read = [None, None]
        cnt3 = 0
        for hf in range(2):
            if hf == 0:
                ua = B.dma("sp", uTh[:, 8:16, :], uTd_v[:, 8:16, 0:1024], uth_s2, [s2a_mm, ut_store[-1]])
                ub = B.dma("sp", uTh[:, 0:8, :], uTd_v[:, 0:8, 0:1024], uth_s, [s2b_mm, ut_store[-1]])
            else:
                ua = B.dma("sp", uTh[:, 8:16, :], uTd_v[:, 8:16, 1024:2048], uth_s2, [s3b_mm])
                ub = B.dma("sp", uTh[:, 0:8, :], uTd_v[:, 0:8, 1024:2048], uth_s, [s3b_mm])
            uth_tok = [ua, ub]
            mgw = [s2b_mm] if hf == 0 else [s2b_mm, wpm_tok]
            for j in range(16):
                cvj, gt = R1.get()
                bt = gt
                gv = cvj[:, 0:4096].rearrange("p (f k c) -> p f k c", f=2, k=16)
                bv = cvj[:, 4096:6144].rearrange("p (f k c) -> p f k c", f=2, k=8)
                for jl in range(1):
                    for tb in range(2):
                        st = cnt3 % 2
                        bks = [4 * st + i for i in range(4)]
                        t0 = hf * 1024 + tb * 512
                        mts = []
                        for q in range(4):
                            bk = bks[q]
                            nk = 16 if q < 2 else 8
                            for k in range(nk):
                                if q < 2:
                                    kk = (k + 8) % 16
                                    f = lambda e, bk=bk, q=q, k=k, kk=kk, jl=jl, tb=tb, gv=gv: e.matmul(
                                        ps[bk][:, :], gv[:, q, kk, 0:128],
                                        uTh[:, kk, tb * 512:(tb + 1) * 512], start=(k == 0), stop=(k == 15))
                                    w = [gt, uth_tok[0] if k < 8 else uth_tok, psfree[bk]]
                                else:
                                    zz = zfmT if q == 2 else zpmT
                                    f = lambda e, bk=bk, q=q, k=k, jl=jl, t0=t0, bv=bv, zz=zz: e.matmul(
                                        ps[bk][:, :], bv[:, q - 2, k, 0:128],
                                        zz[:, k, t0:t0 + 512], start=(k == 0), stop=(k == 7))
                                    w = [bt, z_toks, psfree[bk]]
                                if k < nk - 1:
                                    B.op0("pe", f, w)
                                else:
                                    mts.append(B.op("pe", f))
                        sf = tmpf[:, st * 2 + 0, :]
                        sp_ = tmpf[:, st * 2 + 1, :]
                        a1 = B.op("act", lambda e, sf=sf, bk=bks[0], j=j: e.activation(
                            sf, ps[bk][:, :], AF.Sigmoid, bias=bgate[:, j:j + 1]), [mts[0], tmp_read[st], s2a_mm, ctok])
                        a2 = B.op("act", lambda e, sp_=sp_, bk=bks[1], j=j: e.activation(
                            sp_, ps[bk][:, :], AF.Sigmoid, bias=bgate[:, 16 + j:17 + j]), [mts[1]])
                        d1 = B.op("dve", lambda e, sf=sf, bk=bks[2]: e.tensor_tensor(sf, sf, ps[bk][:, :], ALU.mult),
                                  [a1, mts[2]])
                        d2 = B.op("dve", lambda e, sp_=sp_, bk=bks[3]: e.tensor_tensor(sp_, sp_, ps[bk][:, :], ALU.mult),
                                  [a2, mts[3], d1])
                        d3 = B.op("dve", lambda e, sf=sf, sp_=sp_, hf=hf, j=j, tb=tb: e.tensor_tensor(
                            mg[hf][:, j, tb * 512:(tb + 1) * 512], sf, sp_, ALU.add), [d1, d2] + mgw)
                        psfree[bks[0]] = a1
                        psfree[bks[1]] = a2
                        psfree[bks[2]] = d1
                        psfree[bks[3]] = d2
                        tmp_read[st] = d3
                        cnt3 += 1
                R1.release(mts[3])
            s3b_mm = mts[3]
            mg_toks.append(d3)

        wo_s4 = [B.dsem("wo%d" % i) for i in range(4)]
        woA = bfv(rA[0], 64 * K).rearrange("p (j n) -> p j n", j=16)
        wo_toks = []
        for nb in range(4):
            wo_toks.append(B.dma("pool", woA[:, :, nb * 512:(nb + 1) * 512],
                                 wout[nb].rearrange("p (j n) -> p j n", j=16), wo_s4[nb],
                                 [s3b_mm]))
        conv_total = 16 * len(conv_pieces)
        conv_done = Tok(cvs.sem, conv_total)
        gmoe_s = f32v(rD[0], 2048)
        gms = B.dsem("gms")
        gmoe_tok = B.dma("sp", gmoe_s, gmoe_d, gms, [s3b_mm])
        xb2 = [f32v(rB[0], 2048), f32v(rB[0] + 8 * K, 2048)]
        hb = [f32v(rB[0] + 16 * K, 2048), f32v(rB[0] + 24 * K, 2048)]
        vb = [bfv(rD[0] + 8 * K, 2048), bfv(rD[0] + 12 * K, 2048)]
        vT = [bfv(rD[0] + 16 * K, 2048).rearrange("p (k c) -> p k c", k=16),
              bfv(rD[0] + 20 * K, 2048).rearrange("p (k c) -> p k c", k=16)]
        affT = f32v(rD[0] + 24 * K, 2048)
        x2s = [B.dsem(), B.dsem()]
        hst = [B.dsem(), B.dsem()]
        vst = [B.dsem(), B.dsem()]
        ssq2 = stat[:, 32:48]
        rstd2 = stat[:, 48:64]
        smx = stat[:, 64:80]
        ssm = stat[:, 80:96]
        afv = afftm[:, :].rearrange("p (t e) -> p t e", e=16)
        exb = stat[:, 96:128].rearrange("p (b e) -> p b e", e=16)
        xrd = [None, None]
        h_st = [None, None]
        v_st = [None, None]
        vtr = [None, None]
        lg_mm = [None, None]
        v_all = []
        h_all = []
        ex_rd = [None, None]
        c3 = {}
        wrv = wr_s[:, :].rearrange("p (k e) -> p k e", e=16)
        x2t = {}

        def x2_load(t):
            b = t % 2
            x2t[t] = B.dma("sp", xb2[b], x[t * 128:(t + 1) * 128, :], x2s[b], [xrd[b], s3b_mm])

        def mm_group(t, nb):
            b = t % 2
            hf, tl = t // 8, t % 8
            bk = nb
            if nb == 0:
                if t == 8:
                    B.wait("pool", [c3.get("mm")])
                    conv_emit(6)
            for j in range(16):
                f = lambda e, bk=bk, j=j, hf=hf, tl=tl, nb=nb: e.matmul(
                    ps[bk][:, :], mg[hf][:, j, tl * 128:(tl + 1) * 128], woA[:, j, nb * 512:(nb + 1) * 512],
                    start=(j == 0), stop=(j == 15))
                if j < 15:
                    B.op0("pe", f, [wo_toks[nb], mg_toks, psfree[bk]])
                else:
                    mt = B.op("pe", f)
            ad = B.op("dve", lambda e, bk=bk, nb=nb, b=b: e.tensor_tensor(
                hb[b][:, nb * 512:(nb + 1) * 512], ps[bk][:, :], xb2[b][:, nb * 512:(nb + 1) * 512], ALU.add),
                [mt, x2t[t], h_st[b], s3b_mm])
            psfree[bk] = ad
            c3["mm"] = mt
            if nb == 0:
                c3["adds"] = []
            c3["adds"].append(ad)
            if nb == 3:
                xrd[b] = ad
                if t + 1 < 16:
                    x2_load(t + 1)

        def norm_v(t):
            b = t % 2
            adds = c3["adds"]
            sq = B.op("act", lambda e, b=b, t=t: e.activation(vb[b], hb[b], AF.Square, accum_out=ssq2[:, t:t + 1]),
                      [adds, v_st[b], vtr[b], s3b_mm])
            r1 = B.op("act", lambda e, t=t: e.activation(rstd2[:, t:t + 1], ssq2[:, t:t + 1], AF.Sqrt, bias=EPS,
                                                        scale=1.0 / D), [sq])
            r2 = B.op("dve", lambda e, t=t: e.reciprocal(rstd2[:, t:t + 1], rstd2[:, t:t + 1]), [r1])
            vv = B.op("dve", lambda e, b=b, t=t: e.scalar_tensor_tensor(
                vb[b], hb[b], rstd2[:, t:t + 1], gmoe_s, ALU.mult, ALU.mult), [r2, gmoe_tok, sq])
            c3["vv%d" % t] = vv
            c3["vv"] = vv
            h_st[b] = B.dma("sp", hbuf[t * 128:(t + 1) * 128, :], hb[b], hst[b], [adds, sq])
            v_st[b] = B.dma("sp", vbuf[t * 128:(t + 1) * 128, :], vb[b], vst[b], [vv])
            h_all.append(h_st[b])
            v_all.append(v_st[b])

        def st_tr(t):
            b = t % 2
            vv = c3["vv%d" % t]
            for k in range(16):
                bk = 4 + k // 8
                f = lambda e, bk=bk, k=k, b=b: e.transpose(
                    psb(bk)[:, (k % 8) * 128:(k % 8 + 1) * 128], vb[b][:, k * 128:(k + 1) * 128], identb[:, :])
                if k < 15:
                    B.op0("pe", f, [vv, psfree[4], psfree[5]])
                else:
                    tr = B.op("pe", f)
            vtr[b] = tr
            e1 = B.op("act", lambda e, b=b: e.activation(
                vT[b][:, 0:8, :], psb(4).rearrange("p (k c) -> p k c", k=8), AF.Copy), [tr, lg_mm[b]])
            e2 = B.op("act", lambda e, b=b: e.activation(
                vT[b][:, 8:16, :], psb(5).rearrange("p (k c) -> p k c", k=8), AF.Copy), [tr])
            psfree[4] = e1
            psfree[5] = e2
            c3["e12"] = [e1, e2]

        def st_lg(t):
            b = t % 2
            for k in range(16):
                f = lambda e, k=k, b=b: e.matmul(ps[6][:, 0:16], vT[b][:, k, :], wrv[:, k, :],
                                                 start=(k == 0), stop=(k == 15))
                if k < 15:
                    B.op0("pe", f, c3["e12"] + [wr_tok, psfree[6]])
                else:
                    lm = B.op("pe", f)
            lg_mm[b] = lm
            c3["lm"] = lm
            m1 = B.op("dve", lambda e, t=t: e.reduce_max(smx[:, t:t + 1], ps[6][:, 0:16], AX.X), [lm])
            m2 = B.op("dve", lambda e, t=t: e.tensor_scalar(smx[:, t:t + 1], smx[:, t:t + 1], -1.0, None, ALU.mult),
                      [m1])
            ex = B.op("act", lambda e, t=t, b=b: e.activation(
                exb[:, b, :], ps[6][:, 0:16], AF.Exp, bias=smx[:, t:t + 1], accum_out=ssm[:, t:t + 1]),
                [m2, ex_rd[b]])
            psfree[6] = ex
            r3 = B.op("dve", lambda e, t=t: e.reciprocal(ssm[:, t:t + 1], ssm[:, t:t + 1]), [ex])
            af = B.op("dve", lambda e, t=t, b=b: e.tensor_scalar(
                afv[:, t, :], exb[:, b, :], ssm[:, t:t + 1], None, ALU.mult), [r3])
            ex_rd[b] = af
            c3["af"] = af

        def st_at(t):
            tq = t % 4
            at = B.op("pe", lambda e, t=t, tq=tq: e.transpose(
                ps[7][0:16, tq * 128:(tq + 1) * 128], afv[:, t, :], identf[:, :]),
                [c3["af"], psfree[7] if tq == 0 else None])
            if tq == 3:
                q4 = t // 4
                c3["aff_ev"] = B.op("act", lambda e, q4=q4: e.activation(
                    affT[0:16, q4 * 512:(q4 + 1) * 512], ps[7][0:16, :], AF.Copy), [at, s3b_mm])
                psfree[7] = c3["aff_ev"]

        x2_load(0)
        for t in range(17):
            if t < 16:
                mm_group(t, 0)
            if t >= 2:
                st_at(t - 2)
            if t < 16:
                mm_group(t, 1)
            if t >= 1:
                st_tr(t - 1)
            if t < 16:
                mm_group(t, 2)
            if t >= 1:
                st_lg(t - 1)
            if t < 16:
                mm_group(t, 3)
                norm_v(t)
        st_at(15)
        s3c_mm = c3["mm"]
        adds = c3["adds"]
        vv = c3["vv"]
        lm = c3["lm"]
        aff_ev = c3["aff_ev"]

        r2_slots = [bfv(rA[0] + i * 8 * K, 8 * K) for i in range(12)]
        ch2 = []
        bf_set = set()
        for e_ in range(NE):
            ci = e_ - (NE - NCONV)
            for ft in range(NFT):
                if ci >= 0:
                    bf_set.add(len(ch2))
                    ch2.append((egu_bf[ci, ft], 4096))
                elif e_ == E_PART and ft < NPART:
                    bf_set.add(len(ch2))
                    ch2.append((egu_bf[NCONV, ft], 4096))
                else:
                    ch2.append((egu[e_, ft], 4096))
            for nq in range(8):
                if ci >= 0:
                    bf_set.add(len(ch2))
                ch2.append((edn[e_, nq] if ci < 0 else edn_bf[ci, nq], 2816))
        R2 = Stream(B, r2_slots, ch2, start_waits=[s3c_mm],
                    extra_waits={min(bf_set): [conv_done]}, hwdge_from=bf_set,
                    after_issue=lambda j_: conv_emit(1000 if j_ == 11 else 0))
        R2.prefetch()

        gfin_s = f32v(rD[0], 2048)
        gfs = B.dsem("gfs")
        gfin_tok = B.dma("sp", gfin_s, gfin_d, gfs, [vv])
        work = f32v(rB[0], 2048)
        vals = f32v(rB[0] + 8 * K, 256)
        idxs = arena[:, rB[0] + 9 * K: rB[0] + 9 * K + 512].bitcast(U32)
        idxf = f32v(rB[0] + 10 * K, 256)
        w0 = B.op("dve", lambda e: e.tensor_copy(work[0:16, :], affT[0:16, :]), [aff_ev, adds, xrd[0], xrd[1]])
        prev = w0
        for it in range(32):
            sl = slice(it * 8, it * 8 + 8)
            a = B.op("dve", lambda e, sl=sl: e.max(vals[0:16, sl], work[0:16, :]), [prev])
            bq = B.op("dve", lambda e, sl=sl: e.max_index(idxs[0:16, sl], vals[0:16, sl], work[0:16, :]), [a])
            prev = B.op("dve", lambda e, sl=sl: e.match_replace(work[0:16, :], vals[0:16, sl], work[0:16, :], -1.0),
                        [bq])
        cf = B.op("dve", lambda e: e.tensor_copy(idxf[0:16, :], idxs[0:16, :]), [prev])
        for ct in range(2):
            B.op0("pe", lambda e, ct=ct: e.transpose(
                ps[0][:, ct * 16:(ct + 1) * 16], idxf[0:16, ct * 128:(ct + 1) * 128], identf[0:16, 0:16]),
                [cf, psfree[0]])
            trk = B.op("pe", lambda e, ct=ct: e.transpose(
                ps[0][:, 32 + ct * 16:32 + (ct + 1) * 16], vals[0:16, ct * 128:(ct + 1) * 128], identf[0:16, 0:16]))
        i1 = B.op("dve", lambda e: e.tensor_copy(idxT[:, :], ps[0][:, 0:32]), [trk])
        i2 = B.op("dve", lambda e: e.tensor_copy(gateT[:, :], ps[0][:, 32:64]), [trk, i1])
        psfree[0] = i2
        route_tok = i2
        idxTv = idxT[:, :].rearrange("p (c e) -> p c e", e=16)
        gateTv = gateT[:, :].rearrange("p (c e) -> p c e", e=16)

        xs = [bfv(rB[0] + 16 * K, 4096).rearrange("p (c d) -> p c d", c=2),
              bfv(rB[0] + 24 * K, 4096).rearrange("p (c d) -> p c d", c=2)]
        xsT = [bfv(rB[0] + 32 * K, 4096).rearrange("p (k c) -> p k c", k=16),
               bfv(rB[0] + 40 * K, 4096).rearrange("p (k c) -> p k c", k=16)]
        h1T = [bfv(rB[0] + 48 * K, 2816).rearrange("p (f c) -> p f c", f=11),
               bfv(rB[0] + 54 * K, 2816).rearrange("p (f c) -> p f c", f=11)]
        ybuf = [f32v(rD[0] + 8 * K, 2048), f32v(rD[0] + 16 * K, 2048)]
        silt = [f32v(rB[0] + 60 * K, 256), f32v(rB[0] + 61 * K, 256)]
        gsem = [B.dsem(), B.dsem()]
        scs = B.dsem("scs")
        g_tok = [None, None]
        xs_rd = [None, None]
        xsT_rd = [None, None]
        h1_rd = [None, None]
        sc_tok = None
        y_rd = [None, None]
        sil_rd = [None, None]
        moe_pre = [route_tok, cf, prev] + v_all + h_all + [aff_ev]

        def gather(e_):
            b = e_ % 2
            tk = None
            for ct in range(2):
                tk = B.raw("pool", lambda e, ct=ct, b=b, e_=e_: e.indirect_dma_start(
                    out=xs[b][:, ct, :], out_offset=None, in_=vbuf[:, :],
                    in_offset=bass.IndirectOffsetOnAxis(ap=idxTv[:, ct, e_:e_ + 1], axis=0)),
                    gsem[b], moe_pre + [xs_rd[b], lm, s3c_mm])
            g_tok[b] = tk

        gather(0)
        evn = 0
        for e_ in range(NE):
            b = e_ % 2
            if e_ + 1 < NE:
                gather(e_ + 1)
            for q in range(4):
                bk = q % 2
                for kk in range(4):
                    k = q * 4 + kk
                    for ct in range(2):
                        f = lambda e, bk=bk, kk=kk, ct=ct, k=k, b=b: e.transpose(
                            psb(bk)[:, kk * 256 + ct * 128: kk * 256 + (ct + 1) * 128],
                            xs[b][:, ct, k * 128:(k + 1) * 128], identb[:, :])
                        if kk == 3 and ct == 1:
                            tr = B.op("pe", f)
                        else:
                            B.op0("pe", f, [g_tok[b], psfree[bk], i2])
                w = [tr, xsT_rd[b]]
                if q % 2 == 0:
                    ev = B.op("act", lambda e, bk=bk, q=q, b=b: e.activation(
                        xsT[b][:, q * 4:(q + 1) * 4, :], psb(bk).rearrange("p (k c) -> p k c", k=4), AF.Copy), w)
                else:
                    ev = B.op("dve", lambda e, bk=bk, q=q, b=b: e.tensor_copy(
                        xsT[b][:, q * 4:(q + 1) * 4, :], psb(bk).rearrange("p (k c) -> p k c", k=4)), w)
                psfree[bk] = ev
                if q == 2:
                    xt_a = ev
                if q == 3:
                    xt_d = ev
            xs_rd[b] = tr
            xsT_w = [xt_a, xt_d]
            hl = {}
            for ft in range(NFT):
                cv, ctk = R2.get()
                cv = cv.rearrange("p (g k c) -> p g k c", g=2, k=16)
                st = ft % 2
                bg, bu = 2 + 2 * st, 3 + 2 * st
                for gu in range(2):
                    bk = bg if gu == 0 else bu
                    for k in range(16):
                        f = lambda e, bk=bk, gu=gu, k=k, cv=cv, b=b: e.matmul(
                            ps[bk][:, 0:256], cv[:, gu, k, :], xsT[b][:, k, :], start=(k == 0), stop=(k == 15))
                        if k < 15:
                            B.op0("pe", f, [ctk, xsT_w, psfree[bk]])
                        else:
                            mt = B.op("pe", f)
                    if gu == 0:
                        mg_ = mt
                R2.release(mt)
                sl_ = silt[st]
                a1 = B.op("act", lambda e, sl_=sl_, bg=bg: e.activation(sl_, ps[bg][:, 0:256], AF.Silu),
                          [mg_, sil_rd[st]])
                d1 = B.op("dve", lambda e, sl_=sl_, bu=bu, ft=ft, b=b: e.tensor_tensor(
                    h1T[b][:, ft, :], sl_, ps[bu][:, 0:256], ALU.mult), [a1, mt, h1_rd[b]])
                psfree[bg] = a1
                psfree[bu] = d1
                sil_rd[st] = d1
                hl[ft] = d1
            xsT_rd[b] = mt
            ylast = [[], []]
            for nq in range(8):
                cv, ctk = R2.get()
                cv = cv.rearrange("p (f n) -> p f n", f=11)
                for ct in range(2):
                    bk = 6 + ct
                    for ft in range(NFT):
                        f = lambda e, bk=bk, ft=ft, ct=ct, cv=cv, b=b: e.matmul(
                            ps[bk][:, 0:256], h1T[b][:, ft, ct * 128:(ct + 1) * 128], cv[:, ft, :],
                            start=(ft == 0), stop=(ft == NFT - 1))
                        if ft < NFT - 1:
                            B.op0("pe", f, [ctk, hl[NFT - 1], hl[NFT - 2], psfree[bk]])
                        else:
                            mt = B.op("pe", f)
                    w = [mt, y_rd[ct], route_tok] + (v_all if e_ == 0 else [])
                    if ct == 0:
                        ev = B.op("act", lambda e, bk=bk, ct=ct, nq=nq, e_=e_: e.activation(
                            ybuf[ct][:, nq * 256:(nq + 1) * 256], ps[bk][:, 0:256], AF.Copy,
                            scale=gateTv[:, ct, e_:e_ + 1]), w)
                    else:
                        ev = B.op("dve", lambda e, bk=bk, ct=ct, nq=nq, e_=e_: e.tensor_scalar(
                            ybuf[ct][:, nq * 256:(nq + 1) * 256], ps[bk][:, 0:256], gateTv[:, ct, e_:e_ + 1],
                            None, ALU.mult), w)
                    psfree[bk] = ev
                    ylast[ct] = ev
                R2.release(mt)
            h1_rd[b] = mt
            moe_mm = mt
            for ct in range(2):
                sc_tok = B.raw("pool", lambda e, ct=ct, e_=e_: e.indirect_dma_start(
                    out=hbuf[:, :], out_offset=bass.IndirectOffsetOnAxis(ap=idxTv[:, ct, e_:e_ + 1], axis=0),
                    in_=ybuf[ct][:, :], in_offset=None, compute_op=ALU.add),
                    scs, [ylast[ct], sc_tok] + h_all)
                y_rd[ct] = sc_tok

        NB6 = 4
        fin = [f32v(rA[0] + i * 8 * K, 2048) for i in range(NB6)]
        fo = [f32v(rA[0] + (NB6 + i) * 8 * K, 2048) for i in range(NB6)]
        fjunk = bfv(rA[0] + 2 * NB6 * 8 * K, 2048)
        fls = [B.dsem() for _ in range(NB6)]
        fss = [B.dsem() for _ in range(NB6)]
        ssq3 = stat[:, 0:16]
        rstd3 = stat[:, 16:32]
        f_rd = [None] * NB6
        o_st = [None] * NB6
        o_all = []
        sqp = None
        lts = {}

        def f_load(t):
            b = t % NB6
            lts[t] = B.dma("sp", fin[b], hbuf[t * 128:(t + 1) * 128, :], fls[b], [sc_tok, moe_mm, f_rd[b]])

        for t in range(NB6):
            f_load(t)
        for t in range(16):
            b = t % NB6
            lt = lts[t]
            sq = B.op("act", lambda e, b=b, t=t: e.activation(fjunk, fin[b], AF.Square, accum_out=ssq3[:, t:t + 1]),
                      [lt, moe_mm, sqp])
            sqp = sq
            r1 = B.op("act", lambda e, t=t: e.activation(rstd3[:, t:t + 1], ssq3[:, t:t + 1], AF.Sqrt, bias=EPS,
                                                        scale=1.0 / D), [sq])
            r2 = B.op("dve", lambda e, t=t: e.reciprocal(rstd3[:, t:t + 1], rstd3[:, t:t + 1]), [r1])
            oo = B.op("dve", lambda e, b=b, t=t: e.scalar_tensor_tensor(
                fo[b], fin[b], rstd3[:, t:t + 1], gfin_s, ALU.mult, ALU.mult), [r2, gfin_tok, o_st[b], moe_mm])
            f_rd[b] = oo
            if t + NB6 < 16:
                f_load(t + NB6)
            o_st[b] = B.dma("sp", out[t * 128:(t + 1) * 128, :], fo[b], fss[b], [oo])
            o_all.append(o_st[b])
        B.wait("sp", o_all[-NB6:])
        if debug:
            dbs = B.dsem("dbs")
            B.wait("sp", [i2])
            t1 = B.dma("sp", dbg[:, 0:256], afftm[:, :], dbs)
            t2 = B.dma("sp", dbg[:, 256:288], gateT[:, :], dbs)
            t3 = B.dma("sp", dbg[:, 288:320], idxT[:, :].bitcast(F32), dbs)
            B.wait("sp", [t3])

        with nc.Block() as block:
            @block.tensor
            def _(e):
                for f in B.prog["pe"]:
                    f(e)

            @block.scalar
            def _(e):
                for f in B.prog["act"]:
                    f(e)

            @block.vector
            def _(e):
                for f in B.prog["dve"]:
                    f(e)

            @block.gpsimd
            def _(e):
                for f in B.prog["pool"]:
                    f(e)

            @block.sync
            def _(e):
                for f in B.prog["sp"]:
                    f(e)
    return nc


_CONST = {}


def _constants():
    if _CONST:
        return _CONST
    bf = ml_dtypes.bfloat16
    n = np.arange(S, dtype=np.float64)
    ang = 2.0 * np.pi * ((n[:, None] * n[None, :]) % S) / S
    dft = np.empty((2, 2, 128, 8192), dtype=bf)
    for c_s, m in enumerate((np.cos(ang), np.sin(ang))):
        mb = m.astype(np.float32).astype(bf)
        for kb in range(2):
            blk = mb[:, kb * 512:(kb + 1) * 512].reshape(16, 128, 512).transpose(1, 0, 2)
            dft[kb, c_s] = blk.reshape(128, 8192)
    c = np.arange(256, dtype=np.float64)
    ang2 = 2.0 * np.pi * ((c[:, None] * c[None, :]) % 256) / 256
    ccsc = np.empty((128, 2, 2, 256), dtype=bf)
    for c_s, m in enumerate((np.cos(ang2), np.sin(ang2))):
        ccsc[:, c_s] = m.astype(np.float32).astype(bf).reshape(2, 128, 256).transpose(1, 0, 2)
    poolr = np.zeros((4, 2, 128, 2, 6, 512), dtype=np.float32)
    i = np.arange(S)
    for g, w in enumerate((2, 4, 8, 16)):
        half = w // 2
        lo = np.clip(i - half, 0, S)
        hi = np.clip(i + half, 0, S)
        P = np.zeros((S, S), dtype=np.float64)
        for ii in range(S):
            P[ii, lo[ii]:hi[ii]] = 1.0 / (hi[ii] - lo[ii])
            P[ii, ii] -= 1.0
        PT = P.T
        for ib in range(4):
            for jj in range(6):
                jt = 4 * ib - 1 + jj
                if 0 <= jt < 16:
                    poolr[ib, g // 2, :, g % 2, jj, :] = PT[jt * 128:(jt + 1) * 128, ib * 512:(ib + 1) * 512]
    _CONST["dft"] = dft
    _CONST["ccsc"] = ccsc.reshape(128, 1024)
    _CONST["poolr"] = poolr.astype(bf).reshape(4, 2, 128, 6144)
    _CONST["alt"] = np.tile(np.where(np.arange(128) % 2 == 0, 1.0, -1.0).astype(np.float32)[:, None], (1, 16)).astype(bf)
    _CONST["identb"] = np.eye(128, dtype=np.float32).astype(bf)
    _CONST["identf"] = np.eye(128, dtype=np.float32)
    return _CONST


def _layout_weights(inp):
    f = lambda a: np.ascontiguousarray(np.asarray(a, dtype=np.float32))
    W = {}
    w_in = f(inp["w_in"])[0]
    W["win"] = w_in.reshape(16, 128, 4, 512).transpose(2, 1, 0, 3).reshape(4, 128, 8192).copy()
    wg = f(inp["w_gate"])[0].reshape(16, 128, 2, 16, 128)
    wbf = f(inp["w_branch_f"])[0].reshape(8, 128, 16, 128)
    wbp = f(inp["w_branch_p"])[0].reshape(8, 128, 16, 128)
    wmix = np.empty((16, 128, 6144), dtype=np.float32)
    wmix[:, :, 0:4096] = wg.transpose(3, 1, 2, 0, 4).reshape(16, 128, 4096)
    wmix[:, :, 4096:5120] = wbf.transpose(2, 1, 0, 3).reshape(16, 128, 1024)
    wmix[:, :, 5120:6144] = wbp.transpose(2, 1, 0, 3).reshape(16, 128, 1024)
    W["wmix"] = wmix
    wo = f(inp["w_out"])[0]
    W["wout"] = wo.reshape(16, 128, 4, 512).transpose(2, 1, 0, 3).reshape(4, 128, 8192).copy()
    W["wfm"] = f(inp["w_fourier_mix"])[0].reshape(4, 2, 128, 256).transpose(2, 0, 1, 3).reshape(128, 2048).copy()
    W["wpm"] = f(inp["w_pool_mix"])[0].reshape(4, 2, 128, 256).transpose(2, 0, 1, 3).reshape(128, 2048).copy()
    eg = f(inp["w_expert_gate"])[0].reshape(NE, 16, 128, NFT, 128)
    eu = f(inp["w_expert_up"])[0].reshape(NE, 16, 128, NFT, 128)
    egu = np.empty((NE, NFT, 128, 2, 16, 128), dtype=np.float32)
    egu[:, :, :, 0] = eg.transpose(0, 3, 2, 1, 4)
    egu[:, :, :, 1] = eu.transpose(0, 3, 2, 1, 4)
    W["egu"] = egu.reshape(NE, NFT, 128, 4096)
    ed = f(inp["w_expert_down"])[0].reshape(NE, NFT, 128, 8, 256)
    W["edn"] = ed.transpose(0, 3, 2, 1, 4).reshape(NE, 8, 128, 2816).copy()
    W["gmix"] = f(inp["norm_mix_g"])[0].reshape(16, 128).T.copy()
    W["bgate"] = f(inp["b_gate"])[0].reshape(32, 128).T.copy()
    W["pscale"] = f(inp["pool_scale"])[0].reshape(8, 128).T.copy()
    W["gmoe"] = np.broadcast_to(f(inp["norm_moe_g"])[0][None, :], (128, D)).copy()
    W["gfin"] = np.broadcast_to(f(inp["norm_final_g"])[None, :], (128, D)).copy()
    W["wr"] = f(inp["w_router"])[0].reshape(16, 128, 16).transpose(1, 0, 2).reshape(128, 256).copy()
    W.update(_constants())
    return W


_NC = {}


def kernel(**inputs):
    W = _layout_weights(inputs)
    xin = np.ascontiguousarray(np.asarray(inputs["x"], dtype=np.float32))
    if "nc" not in _NC:
        _NC["nc"] = build_nc()
    nc = _NC["nc"]
    in_maps = []
    for b in range(8):
        m = dict(W)
        m["x"] = xin[b]
        in_maps.append(m)
    res = run_bass_kernel_spmd(nc, in_maps, core_ids=list(range(8)))
    return np.stack([np.asarray(r["out"], dtype=np.float32) for r in res.results], axis=0)
```

```python
import math
from contextlib import ExitStack

import numpy as np
import ml_dtypes
import concourse.bass as bass
import concourse.mybir as mybir
from concourse.bass_utils import run_bass_kernel_spmd

F32 = mybir.dt.float32
BF16 = mybir.dt.bfloat16
I32 = mybir.dt.int32
U32 = mybir.dt.uint32
AF = mybir.ActivationFunctionType
ALU = mybir.AluOpType
AX = mybir.AxisListType

S = 2048
D = 2048
NE = 16
FF = 1408
NFT = 11
CAP = 256
EPS = 1e-6
ENGS = ("pe", "act", "dve", "pool", "sp")


class Tok:
    __slots__ = ("sem", "val")

    def __init__(self, sem, val):
        self.sem = sem
        self.val = val


class DSem:
    def __init__(self, sem):
        self.sem = sem
        self.n = 0


class Builder:
    def __init__(self, nc, es):
        self.nc = nc
        self.es = es
        self.prog = {e: [] for e in ENGS}
        self.cnt = {e: 0 for e in ENGS}
        self.waited = {}
        self.done = {e: es.enter_context(nc.semaphore("done_" + e)) for e in ENGS}
        self.nsem = 0

    def dsem(self, name=None):
        self.nsem += 1
        return DSem(self.es.enter_context(self.nc.semaphore(name or ("ds%d" % self.nsem))))

    def wait(self, eng, toks):
        for t in toks:
            if t is None:
                continue
            if isinstance(t, (list, tuple)):
                self.wait(eng, t)
                continue
            key = (eng, id(t.sem))
            if self.waited.get(key, 0) >= t.val:
                continue
            self.waited[key] = t.val
            self.prog[eng].append(lambda e, t=t: e.wait_ge(t.sem, t.val))

    def op(self, eng, fn, waits=()):
        self.wait(eng, waits)
        self.cnt[eng] += 1
        n = self.cnt[eng]
        s = self.done[eng]
        self.prog[eng].append(lambda e: fn(e).then_inc(s, 1))
        return Tok(s, n)

    def op0(self, eng, fn, waits=()):
        self.wait(eng, waits)
        self.prog[eng].append(lambda e: fn(e))

    def dma(self, eng, out, in_, ds, waits=(), **kw):
        self.wait(eng, waits)
        ds.n += 16
        v = ds.n
        self.prog[eng].append(lambda e: e.dma_start(out=out, in_=in_, **kw).then_inc(ds.sem, 16))
        return Tok(ds.sem, v)

    def raw(self, eng, fn, ds, waits=()):
        self.wait(eng, waits)
        ds.n += 16
        v = ds.n
        self.prog[eng].append(lambda e: fn(e).then_inc(ds.sem, 16))
        return Tok(ds.sem, v)


class Stream:
    def __init__(self, B, slots, chunks, start_waits=(), after_issue=None, extra_waits=None, hwdge_from=None):
        self.hwdge_from = hwdge_from
        self.after_issue = after_issue
        self.extra_waits = extra_waits or {}
        self.B = B
        self.slots = slots
        self.ns = len(slots)
        self.chunks = chunks
        self.sems = [B.dsem() for _ in slots]
        self.issued = 0
        self.cur = 0
        self.rel = []
        self.toks = []
        self.start_waits = list(start_waits)

    def _issue(self, j):
        src, nel = self.chunks[j]
        sl = j % self.ns
        waits = list(self.start_waits) if j < self.ns else [self.rel[j - self.ns]]
        dst = self.slots[sl][:, 0:nel]
        waits = waits + list(self.extra_waits.get(j, []))
        eng = "pool"
        if self.hwdge_from is not None and j >= self.hwdge_from:
            eng = "sp"
        self.toks.append(self.B.dma(eng, dst, src, self.sems[sl], waits))
        if self.after_issue is not None:
            self.after_issue(j)

    def get(self):
        hi = min(self.cur + self.ns - 1, len(self.chunks) - 1)
        while self.issued <= hi and (self.issued < self.ns or self.issued - self.ns < len(self.rel)):
            self._issue(self.issued)
            self.issued += 1
        j = self.cur
        assert j < self.issued, "stream chunk not issued (ring deadlock)"
        self.cur += 1
        nel = self.chunks[j][1]
        return self.slots[j % self.ns][:, 0:nel], self.toks[j]

    def release(self, tok):
        self.rel.append(tok)

    def prefetch(self):
        while self.issued < min(self.ns, len(self.chunks)):
            self._issue(self.issued)
            self.issued += 1


def build_nc(stage=99, debug=False):
    nc = bass.Bass("TRN2", target_bir_lowering=False)

    def din(name, shape, dt=F32):
        return nc.dram_tensor(name, shape, dt, kind="ExternalInput").ap()

    x = din("x", [S, D])
    win = din("win", [4, 128, 8192])
    wmix = din("wmix", [16, 128, 6144])
    wout = din("wout", [4, 128, 8192])
    wfm = din("wfm", [128, 2048])
    wpm = din("wpm", [128, 2048])
    egu = din("egu", [NE, NFT, 128, 4096])
    edn = din("edn", [NE, 8, 128, 2816])
    dft = din("dft", [2, 2, 128, 8192], BF16)
    ccsc = din("ccsc", [128, 1024], BF16)
    poolr = din("poolr", [4, 2, 128, 6144], BF16)
    gmix_d = din("gmix", [128, 16])
    bgate_d = din("bgate", [128, 32])
    pscale_d = din("pscale", [128, 8])
    gmoe_d = din("gmoe", [128, D])
    gfin_d = din("gfin", [128, D])
    wr_d = din("wr", [128, 256])
    identb_d = din("identb", [128, 128], BF16)
    alt_d = din("alt", [128, 16], BF16)
    identf_d = din("identf", [128, 128])
    out = nc.dram_tensor("out", [S, D], F32, kind="ExternalOutput").ap()
    dk = "ExternalOutput" if debug else "Internal"
    uTd = nc.dram_tensor("uTd", [128, 16 * S], BF16, kind=dk).ap()
    hbuf = nc.dram_tensor("hbuf", [S, D], F32, kind=dk).ap()
    vbuf = nc.dram_tensor("vbuf", [S, D], BF16, kind=dk).ap()
    if debug:
        dbg = nc.dram_tensor("dbg", [128, 8192], F32, kind="ExternalOutput").ap()
    NCONV = 4
    egu_bf = nc.dram_tensor("egu_bf", [NCONV, NFT, 128, 4096], BF16, kind="Internal").ap()
    edn_bf = nc.dram_tensor("edn_bf", [NCONV, 8, 128, 2816], BF16, kind="Internal").ap()
    wout_bf = nc.dram_tensor("wout_bf", [4, 128, 8192], BF16, kind="Internal").ap()

    es = ExitStack()
    with es:
        B = Builder(nc, es)
        sb = lambda name, shape, dt: es.enter_context(nc.sbuf_tensor(name, shape, dt))
        K = 512
        arena = sb("arena", [128, 192 * K], BF16)
        rB = (0, 64 * K)
        rA = (64 * K, 128 * K)
        rC = (128 * K, 160 * K)
        rD = (160 * K, 192 * K)

        def bfv(lo, n):
            return arena[:, lo:lo + n]

        def f32v(lo, n):
            return arena[:, lo:lo + 2 * n].bitcast(F32)

        Gc = sb("Gc", [128, 4096], BF16)
        ccsc_s = sb("ccsc_s", [128, 1024], BF16)
        identb = sb("identb_s", [128, 128], BF16)
        identf = sb("identf_s", [128, 128], F32)
        gmix = sb("gmix_s", [128, 16], F32)
        bgate = sb("bgate_s", [128, 32], F32)
        pscale = sb("pscale_s", [128, 8], F32)
        wr_s = sb("wr_s", [128, 256], BF16)
        alt_s = sb("alt_s", [128, 16], BF16)
        y1024 = sb("y1024", [128, 8], BF16)
        stat = sb("stat", [128, 128], F32)
        afftm = sb("afftm", [128, 256], F32)
        idxT = sb("idxT", [128, 32], I32)
        gateT = sb("gateT", [128, 32], F32)
        ps = [es.enter_context(nc.psum_tensor("ps%d" % i, [128, 512], F32)) for i in range(8)]
        psfree = [None] * 8

        def psb(i):
            return ps[i][:, :].bitcast(BF16)

        cs = B.dsem("cs")
        ctoks = []
        for dst, src in ((identb, identb_d), (identf, identf_d), (gmix, gmix_d), (bgate, bgate_d),
                         (pscale, pscale_d), (ccsc_s, ccsc), (alt_s, alt_d)):
            ctoks.append(B.dma("sp", dst[:, :], src, cs))
        ctok = ctoks[-1]
        wrs = B.dsem("wrs")
        wr_tok = B.dma("pool", wr_s[:, :], wr_d, wrs)

        r1_slots = [bfv(rD[0], 16 * K), bfv(rD[0] + 16 * K, 16 * K)]
        ch1 = [(wfm, 2048)]
        for nb in range(4):
            ch1.append((win[nb], 8192))
        for kb in range(2):
            for c_s in range(2):
                ch1.append((dft[kb, c_s], 8192))
        for ib in range(4):
            for gp in range(2):
                ch1.append((poolr[ib, gp], 6144))
        for hf in range(2):
            for j in range(16):
                ch1.append((wmix[j], 6144))
        cvw = B.dsem("cvw")
        cvs = B.dsem("cvs")
        conv_pieces = []
        for ci in range(NCONV):
            e_ = NE - NCONV + ci
            for ft in range(NFT):
                conv_pieces.append((egu_bf[ci, ft], egu[e_, ft], cvs))
            for nq in range(8):
                conv_pieces.append((edn_bf[ci, nq], edn[e_, nq], cvs))
        conv_state = {"i": 0}

        def conv_emit(n):
            while n > 0 and conv_state["i"] < len(conv_pieces):
                dst, src, sm = conv_pieces[conv_state["i"]]
                B.dma("pool", dst, src, sm)
                conv_state["i"] += 1
                n -= 1

        conv_after = {}
        conv_after[4] = 5
        conv_after[5] = 4
        for j_ in range(6, 9):
            conv_after[j_] = 3
        for j_ in range(17, 49):
            conv_after[j_] = 1
        R1 = Stream(B, r1_slots, ch1, after_issue=lambda j_: conv_emit(conv_after.get(j_, 0)))

        wfm_v, wfm_tok = R1.get()
        wfm_v = wfm_v.rearrange("p (g c d) -> p g c d", g=4, c=2)
        ccv = ccsc_s[:, :].rearrange("p (s c d) -> p s c d", s=2, c=2)
        Gv = Gc[:, :].rearrange("p (i d) -> p i d", d=256)
        nrm = 1.0 / math.sqrt(S * 256.0)
        gi = 0
        last = None
        for g in range(4):
            for c_s in range(2):
                for cc in range(2):
                    bk = 4 + gi % 4
                    for kc in range(2):
                        f = lambda e, bk=bk, c_s=c_s, kc=kc, cc=cc, g=g: e.matmul(
                            ps[bk][:, 0:256], ccv[:, c_s, kc, cc * 128:(cc + 1) * 128], wfm_v[:, g, kc, :],
                            start=(kc == 0), stop=(kc == 1))
                        if kc == 0:
                            B.op0("pe", f, [wfm_tok, ctok, psfree[bk]])
                        else:
                            mt = B.op("pe", f)
                    sc = nrm if c_s == 0 else -nrm
                    idx = g * 4 + c_s * 2 + cc
                    last = B.op("act", lambda e, bk=bk, idx=idx, sc=sc: e.activation(
                        Gv[:, idx, :], ps[bk][:, 0:256], AF.Copy, scale=sc), [mt])
                    psfree[bk] = last
                    gi += 1
        R1.release(mt)
        G_tok = last

        uT = bfv(rA[0], 64 * K).rearrange("p (k t) -> p k t", k=16)
        xb = [f32v(rC[0], 2048), f32v(rC[0] + 8 * K, 2048)]
        xn = [bfv(rC[0] + 16 * K, 2048), bfv(rC[0] + 20 * K, 2048)]
        xs_sem = [B.dsem(), B.dsem()]
        ssq = stat[:, 0:16]
        rstd = stat[:, 16:32]
        cp_tok = [None, None]
        tr_tok = [None, None]
        uT_toks = []
        uts = B.dsem("uts")
        ut_store = []
        uTd_v = uTd.rearrange("p (k t) -> p k t", k=16)
        pf = bfv(rB[0], 32 * K).rearrange("p (t c) -> p t c", t=16)
        pp = bfv(rB[0] + 32 * K, 32 * K).rearrange("p (t c) -> p t c", t=16)
        pst = {"evn": 0, "mt": None}
        p_last = {}

        def p_group(nb, t, wv, wt, eng=None):
            dstp = pf if nb < 2 else pp
            c0 = (nb % 2) * 512
            bk = 4 + pst["evn"] % 4
            for k in range(16):
                f = lambda e, bk=bk, k=k, t=t, wv=wv: e.matmul(
                    ps[bk][:, :], uT[:, k, t * 128:(t + 1) * 128], wv[:, k, :], start=(k == 0), stop=(k == 15))
                if k < 15:
                    B.op0("pe", f, [wt, uT_toks[t], psfree[bk]])
                else:
                    mt = B.op("pe", f)
            if eng is None:
                eng = "act" if pst["evn"] % 2 == 0 else "dve"
            if eng == "act":
                ev = B.op("act", lambda e, bk=bk, t=t, dstp=dstp, c0=c0: e.activation(
                    dstp[:, t, c0:c0 + 512], ps[bk][:, :], AF.Copy), [mt])
            else:
                ev = B.op("dve", lambda e, bk=bk, t=t, dstp=dstp, c0=c0: e.tensor_copy(
                    dstp[:, t, c0:c0 + 512], ps[bk][:, :]), [mt])
            psfree[bk] = ev
            p_last[eng] = ev
            pst["evn"] += 1
            pst["mt"] = mt

        w0v, w0t = R1.get()
        w0v = w0v.rearrange("p (k n) -> p k n", k=16)
        w1v, w1t = R1.get()
        w1v = w1v.rearrange("p (k n) -> p k n", k=16)
        xts = {}

        def x_load(t):
            b = t % 2
            xts[t] = B.dma("sp", xb[b], x[t * 128:(t + 1) * 128, :], xs_sem[b], [cp_tok[b]])

        cps = {}

        def norm_chain(t):
            b = t % 2
            xt = xts[t]
            sq = B.op("act", lambda e, b=b, t=t: e.activation(xn[b], xb[b], AF.Square, accum_out=ssq[:, t:t + 1]),
                      [xt, tr_tok[b]])
            r1 = B.op("act", lambda e, t=t: e.activation(rstd[:, t:t + 1], ssq[:, t:t + 1], AF.Sqrt, bias=EPS,
                                                        scale=1.0 / D), [sq])
            r2 = B.op("dve", lambda e, t=t: e.reciprocal(rstd[:, t:t + 1], rstd[:, t:t + 1]), [r1])
            cp = B.op("act", lambda e, b=b, t=t: e.activation(xn[b], xb[b], AF.Copy, scale=rstd[:, t:t + 1]),
                      [r2, sq])
            cp_tok[b] = cp
            cps[t] = cp
            if t + 2 < 16:
                x_load(t + 2)

        x_load(0)
        x_load(1)
        norm_chain(0)
        for t in range(16):
            b = t % 2
            if t + 1 < 16:
                norm_chain(t + 1)
            cp = cps[t]
            bk0 = 0 if b == 0 else 2
            for k in range(16):
                bk = bk0 + k // 8
                f = lambda e, bk=bk, k=k, b=b: e.transpose(
                    psb(bk)[:, (k % 8) * 128:(k % 8 + 1) * 128], xn[b][:, k * 128:(k + 1) * 128], identb[:, :])
                if k < 15:
                    B.op0("pe", f, [cp, ctok, psfree[bk0], psfree[bk0 + 1]])
                else:
                    tr = B.op("pe", f)
            tr_tok[b] = tr
            for hh in range(2):
                bk = bk0 + hh
                ev = B.op("dve", lambda e, bk=bk, hh=hh, t=t: e.tensor_tensor(
                    uT[:, hh * 8:(hh + 1) * 8, t * 128:(t + 1) * 128],
                    psb(bk).rearrange("p (k c) -> p k c", k=8),
                    gmix[:, hh * 8:(hh + 1) * 8].unsqueeze(2).to_broadcast([128, 8, 128]),
                    ALU.mult), [tr, ctok])
                psfree[bk] = ev
            uT_toks.append(ev)
            if t % 4 == 3:
                tg = t // 4
                ut_store.append(B.dma("sp", uTd_v[:, :, tg * 512:(tg + 1) * 512],
                                      uT[:, :, tg * 512:(tg + 1) * 512], uts, [ev]))
            if t >= 1:
                p_group(0, t - 1, w0v, w0t, "dve")
                p_group(1, t - 1, w1v, w1t, "dve")
        s1a_done = [cp_tok[0], cp_tok[1], tr_tok[0], tr_tok[1]]
        p_group(0, 15, w0v, w0t)
        R1.release(pst["mt"])
        p_group(1, 15, w1v, w1t)
        R1.release(pst["mt"])
        for nb in range(2, 4):
            wv, wt = R1.get()
            wv = wv.rearrange("p (k n) -> p k n", k=16)
            for t in range(16):
                p_group(nb, t, wv, wt)
            R1.release(pst["mt"])
        s1b_mm = pst["mt"]
        p_toks = [p_last["act"], p_last["dve"]]

        YT = [bfv(rA[0], 16 * K).rearrange("p (m s k) -> p m s k", m=8, s=2),
              bfv(rA[0] + 16 * K, 16 * K).rearrange("p (m s k) -> p m s k", m=8, s=2)]
        zfmT = bfv(rA[0] + 32 * K, 32 * K).rearrange("p (m t) -> p m t", m=8)
        yt_read = [None, None]
        a_dead = [s1b_mm, ut_store[-1]]
        evn = 0
        zlast = {}
        tmpA = [f32v(rC[0] + 8 * K, 512), f32v(rC[0] + 10 * K, 512)]
        tmpA_rd = [None, None]
        arena_t = arena[:, :].tensor
        pstep = arena[:, :].ap[0][0]

        def rev_cols(m, hi_col, n):
            base = zfmT[:, m, hi_col:hi_col + 1]
            return bass.AP(tensor=arena_t, offset=base.offset, ap=[[pstep, 128], [-1, n]])

        zi = 0
        for kb in range(2):
            yb = kb % 2
            ylast = {}
            for c_s in range(2):
                dv, dt_ = R1.get()
                dv = dv.rearrange("p (c k) -> p c k", c=16)
                for m in range(8):
                    bk = evn % 4
                    for c in range(16):
                        f = lambda e, bk=bk, c=c, m=m, dv=dv: e.matmul(
                            ps[bk][:, :], pf[:, c, m * 128:(m + 1) * 128], dv[:, c, :], start=(c == 0), stop=(c == 15))
                        if c < 15:
                            B.op0("pe", f, [dt_, p_toks, psfree[bk]])
                        else:
                            mt = B.op("pe", f)
                    eng = "act" if evn % 2 == 0 else "dve"
                    w = [mt, yt_read[yb]] + a_dead
                    if eng == "act":
                        ev = B.op("act", lambda e, bk=bk, m=m, c_s=c_s, yb=yb: e.activation(
                            YT[yb][:, m, c_s, :], ps[bk][:, :], AF.Copy), w)
                    else:
                        ev = B.op("dve", lambda e, bk=bk, m=m, c_s=c_s, yb=yb: e.tensor_copy(
                            YT[yb][:, m, c_s, :], ps[bk][:, :]), w)
                    psfree[bk] = ev
                    ylast[eng] = ev
                    evn += 1
                R1.release(mt)
            yw = [ylast["act"], ylast["dve"], G_tok]
            for g in range(4):
                for dtl in range(2):
                    m = g * 2 + dtl
                    ba = 4 + 2 * (zi % 2)
                    bb = ba + 1
                    tb_ = zi % 2
                    mts = []
                    for c_s in range(2):
                        bk = ba if c_s == 0 else bb
                        for cc in range(2):
                            f = lambda e, bk=bk, g=g, c_s=c_s, cc=cc, dtl=dtl, yb=yb: e.matmul(
                                ps[bk][:, :], Gv[:, g * 4 + c_s * 2 + cc, dtl * 128:(dtl + 1) * 128],
                                YT[yb][:, g * 2 + cc, c_s, :], start=(cc == 0), stop=(cc == 1))
                            if cc == 0:
                                B.op0("pe", f, yw + [psfree[bk]])
                            else:
                                mts.append(B.op("pe", f))
                    mt = mts[1]
                    ca = B.op("act", lambda e, ba=ba, tb_=tb_: e.activation(tmpA[tb_], ps[ba][:, :], AF.Copy),
                              [mts[0], tmpA_rd[tb_], s1a_done])
                    d1 = B.op("dve", lambda e, bb=bb, tb_=tb_, m=m, kb=kb: e.tensor_tensor(
                        zfmT[:, m, kb * 512:(kb + 1) * 512], tmpA[tb_], ps[bb][:, :], ALU.add),
                        [ca, mts[1]] + a_dead)
                    if kb == 0:
                        d2 = B.op("dve", lambda e, bb=bb, tb_=tb_, m=m: e.tensor_tensor(
                            rev_cols(m, 2047, 511), tmpA[tb_][:, 1:512], ps[bb][:, 1:512], ALU.subtract), [d1])
                    else:
                        d2 = B.op("dve", lambda e, bb=bb, tb_=tb_, m=m: e.tensor_tensor(
                            rev_cols(m, 1536, 512), tmpA[tb_][:, 0:512], ps[bb][:, 0:512], ALU.subtract), [d1])
                    psfree[ba] = ca
                    psfree[bb] = d2
                    tmpA_rd[tb_] = d2
                    zlast["dve"] = d2
                    zi += 1
            yt_read[yb] = mt
        for m in range(8):
            for c in range(16):
                f = lambda e, c=c, m=m: e.matmul(ps[0][:, m:m + 1], pf[:, c, m * 128:(m + 1) * 128],
                                                 alt_s[:, c:c + 1], start=(c == 0), stop=(c == 15))
                if c < 15 or m < 7:
                    B.op0("pe", f, [p_toks, psfree[0], ctok])
                else:
                    mt = B.op("pe", f)
        yc = B.op("act", lambda e: e.activation(y1024[:, :], ps[0][:, 0:8], AF.Copy), [mt])
        psfree[0] = yc
        for g in range(4):
            for dtl in range(2):
                m = g * 2 + dtl
                for cc in range(2):
                    f = lambda e, g=g, cc=cc, dtl=dtl, m=m: e.matmul(
                        ps[1][:, m:m + 1], Gv[:, g * 4 + cc, dtl * 128:(dtl + 1) * 128],
                        y1024[:, g * 2 + cc:g * 2 + cc + 1], start=(cc == 0), stop=(cc == 1))
                    if m < 7 or cc == 0:
                        B.op0("pe", f, [yc, psfree[1], G_tok])
                    else:
                        mt = B.op("pe", f)
        zc = B.op("act", lambda e: e.activation(zfmT[:, :, 1024], ps[1][:, 0:8], AF.Copy), [mt] + a_dead)
        psfree[1] = zc
        zlast["act"] = zc
        s2a_mm = mt
        zfm_toks = [zlast["act"], zlast["dve"]]

        wpm_s = bfv(rC[0], 2048).rearrange("p (g c d) -> p g c d", g=4, c=2)
        wpms = B.dsem("wpms")
        wpm_tok = B.dma("pool", bfv(rC[0], 2048), wpm, wpms, s1a_done)
        pooledT = [bfv(rB[0], 4 * K * 2).rearrange("p (m k) -> p m k", m=8),
                   bfv(rB[0] + 8 * K, 4 * K * 2).rearrange("p (m k) -> p m k", m=8)]
        zpmT = bfv(rA[0], 32 * K).rearrange("p (m t) -> p m t", m=8)
        pl_read = [None, None]
        evn = 0
        zplast = {}
        for ib in range(4):
            pb = ib % 2
            pl = {}
            for gp in range(2):
                rv, rt = R1.get()
                rv = rv.rearrange("p (g j k) -> p g j k", g=2, j=6)
                for gl in range(2):
                    g = gp * 2 + gl
                    for ct in range(2):
                        bk = evn % 4
                        jjs = [jj for jj in range(6) if 0 <= 4 * ib - 1 + jj < 16]
                        for n, jj in enumerate(jjs):
                            jt = 4 * ib - 1 + jj
                            f = lambda e, bk=bk, jt=jt, g=g, ct=ct, rv=rv, gl=gl, jj=jj, n=n, L=len(jjs): e.matmul(
                                ps[bk][:, :], pp[:, jt, g * 256 + ct * 128:g * 256 + (ct + 1) * 128],
                                rv[:, gl, jj, :], start=(n == 0), stop=(n == L - 1))
                            if n < len(jjs) - 1:
                                B.op0("pe", f, [rt, p_toks, psfree[bk]])
                            else:
                                mt = B.op("pe", f)
                        eng = "act" if evn % 2 == 0 else "dve"
                        w = [mt, pl_read[pb], s2a_mm]
                        if eng == "act":
                            ev = B.op("act", lambda e, bk=bk, g=g, ct=ct, pb=pb: e.activation(
                                pooledT[pb][:, g * 2 + ct, :], ps[bk][:, :], AF.Copy), w)
                        else:
                            ev = B.op("dve", lambda e, bk=bk, g=g, ct=ct, pb=pb: e.tensor_copy(
                                pooledT[pb][:, g * 2 + ct, :], ps[bk][:, :]), w)
                        psfree[bk] = ev
                        pl[eng] = ev
                        evn += 1
                R1.release(mt)
            plw = [pl["act"], pl["dve"], wpm_tok]
            for g in range(4):
                for dtl in range(2):
                    bk = 4 + (g * 2 + dtl) % 4
                    for cc in range(2):
                        f = lambda e, bk=bk, g=g, cc=cc, dtl=dtl, pb=pb: e.matmul(
                            ps[bk][:, :], wpm_s[:, g, cc, dtl * 128:(dtl + 1) * 128],
                            pooledT[pb][:, g * 2 + cc, :], start=(cc == 0), stop=(cc == 1))
                        if cc == 0:
                            B.op0("pe", f, plw + [psfree[bk]])
                        else:
                            mt = B.op("pe", f)
                    m = g * 2 + dtl
                    eng = "act" if m % 2 == 0 else "dve"
                    if eng == "act":
                        ev = B.op("act", lambda e, bk=bk, m=m, ib=ib: e.activation(
                            zpmT[:, m, ib * 512:(ib + 1) * 512], ps[bk][:, :], AF.Copy, scale=pscale[:, m:m + 1]),
                            [mt, s2a_mm, ctok])
                    else:
                        ev = B.op("dve", lambda e, bk=bk, m=m, ib=ib: e.tensor_scalar(
                            zpmT[:, m, ib * 512:(ib + 1) * 512], ps[bk][:, :], pscale[:, m:m + 1], None, ALU.mult),
                            [mt, s2a_mm, ctok])
                    psfree[bk] = ev
                    zplast[eng] = ev
            pl_read[pb] = mt
        s2b_mm = mt
        z_toks = zfm_toks + [zplast["act"], zplast["dve"]]

        uTh = bfv(rB[0], 32 * K).rearrange("p (k t) -> p k t", k=16)
        mg = [bfv(rB[0] + 32 * K, 32 * K).rearrange("p (j t) -> p j t", j=16),
              bfv(rC[0], 32 * K).rearrange("p (j t) -> p j t", j=16)]
        tmpf = Gc[:, :].bitcast(F32).rearrange("p (i k) -> p i k", k=512)
        uth_s = B.dsem("uth")
        uth_s2 = B.dsem("uth2")
        uTd_v = uTd.rearrange("p (k t) -> p k t", k=16)
        s3b_mm = None
        mg_toks = []
        tmp_read = [None, None]
        cnt3 = 0
        for hf in range(2):
            if hf == 0:
                ua = B.dma("sp", uTh[:, 8:16, :], uTd_v[:, 8:16, 0:1024], uth_s2, [s2a_mm, ut_store[-1]])
                ub = B.dma("sp", uTh[:, 0:8, :], uTd_v[:, 0:8, 0:1024], uth_s, [s2b_mm, ut_store[-1]])
            else:
                ua = B.dma("sp", uTh[:, 8:16, :], uTd_v[:, 8:16, 1024:2048], uth_s2, [s3b_mm])
                ub = B.dma("sp", uTh[:, 0:8, :], uTd_v[:, 0:8, 1024:2048], uth_s, [s3b_mm])
            uth_tok = [ua, ub]
            mgw = [s2b_mm] if hf == 0 else [s2b_mm, wpm_tok]
            for j in range(16):
                cvj, gt = R1.get()
                bt = gt
                gv = cvj[:, 0:4096].rearrange("p (f k c) -> p f k c", f=2, k=16)
                bv = cvj[:, 4096:6144].rearrange("p (f k c) -> p f k c", f=2, k=8)
                for jl in range(1):
                    for tb in range(2):
                        st = cnt3 % 2
                        bks = [4 * st + i for i in range(4)]
                        t0 = hf * 1024 + tb * 512
                        mq = {}
                        for q in (2, 3, 0, 1):
                            bk = bks[q]
                            nk = 16 if q < 2 else 8
                            for k in range(nk):
                                if q < 2:
                                    kk = (k + 8) % 16
                                    f = lambda e, bk=bk, q=q, k=k, kk=kk, jl=jl, tb=tb, gv=gv: e.matmul(
                                        ps[bk][:, :], gv[:, q, kk, 0:128],
                                        uTh[:, kk, tb * 512:(tb + 1) * 512], start=(k == 0), stop=(k == 15))
                                    w = [gt, uth_tok[0] if k < 8 else uth_tok, psfree[bk]]
                                else:
                                    zz = zfmT if q == 2 else zpmT
                                    f = lambda e, bk=bk, q=q, k=k, jl=jl, t0=t0, bv=bv, zz=zz: e.matmul(
                                        ps[bk][:, :], bv[:, q - 2, k, 0:128],
                                        zz[:, k, t0:t0 + 512], start=(k == 0), stop=(k == 7))
                                    w = [bt, z_toks, psfree[bk]]
                                if k < nk - 1:
                                    B.op0("pe", f, w)
                                else:
                                    mq[q] = B.op("pe", f)
                        mts = [mq[0], mq[1], mq[2], mq[3]]
                        last_mm = mq[1]
                        sf = tmpf[:, st * 2 + 0, :]
                        sp_ = tmpf[:, st * 2 + 1, :]
                        a1 = B.op("act", lambda e, sf=sf, bk=bks[0], j=j: e.activation(
                            sf, ps[bk][:, :], AF.Sigmoid, bias=bgate[:, j:j + 1]), [mts[0], tmp_read[st], s2a_mm, ctok])
                        a2 = B.op("act", lambda e, sp_=sp_, bk=bks[1], j=j: e.activation(
                            sp_, ps[bk][:, :], AF.Sigmoid, bias=bgate[:, 16 + j:17 + j]), [mts[1]])
                        d1 = B.op("dve", lambda e, sf=sf, bk=bks[2]: e.tensor_tensor(sf, sf, ps[bk][:, :], ALU.mult),
                                  [a1, mts[2]])
                        d2 = B.op("dve", lambda e, sp_=sp_, bk=bks[3]: e.tensor_tensor(sp_, sp_, ps[bk][:, :], ALU.mult),
                                  [a2, mts[3], d1])
                        d3 = B.op("dve", lambda e, sf=sf, sp_=sp_, hf=hf, j=j, tb=tb: e.tensor_tensor(
                            mg[hf][:, j, tb * 512:(tb + 1) * 512], sf, sp_, ALU.add), [d1, d2] + mgw)
                        psfree[bks[0]] = a1
                        psfree[bks[1]] = a2
                        psfree[bks[2]] = d1
                        psfree[bks[3]] = d2
                        tmp_read[st] = d3
                        cnt3 += 1
                R1.release(last_mm)
            s3b_mm = last_mm
            mg_toks.append(d3)

        wo_s4 = [B.dsem("wo%d" % i) for i in range(4)]
        woA = bfv(rA[0], 64 * K).rearrange("p (j n) -> p j n", j=16)
        wo_toks = []
        for nb in range(4):
            wo_toks.append(B.dma("pool", woA[:, :, nb * 512:(nb + 1) * 512],
                                 wout[nb].rearrange("p (j n) -> p j n", j=16), wo_s4[nb],
                                 [s3b_mm]))
        conv_total = 16 * len(conv_pieces)
        conv_done = Tok(cvs.sem, conv_total)
        gmoe_s = f32v(rD[0], 2048)
        gms = B.dsem("gms")
        gmoe_tok = B.dma("sp", gmoe_s, gmoe_d, gms, [s3b_mm])
        xb2 = [f32v(rB[0], 2048), f32v(rB[0] + 8 * K, 2048)]
        hb = [f32v(rB[0] + 16 * K, 2048), f32v(rB[0] + 24 * K, 2048)]
        vb = [bfv(rD[0] + 8 * K, 2048), bfv(rD[0] + 12 * K, 2048)]
        vT = [bfv(rD[0] + 16 * K, 2048).rearrange("p (k c) -> p k c", k=16),
              bfv(rD[0] + 20 * K, 2048).rearrange("p (k c) -> p k c", k=16)]
        affT = f32v(rD[0] + 24 * K, 2048)
        x2s = [B.dsem(), B.dsem()]
        hst = [B.dsem(), B.dsem()]
        vst = [B.dsem(), B.dsem()]
        ssq2 = stat[:, 32:48]
        rstd2 = stat[:, 48:64]
        smx = stat[:, 64:80]
        ssm = stat[:, 80:96]
        afv = afftm[:, :].rearrange("p (t e) -> p t e", e=16)
        exb = stat[:, 96:128].rearrange("p (b e) -> p b e", e=16)
        xrd = [None, None]
        h_st = [None, None]
        v_st = [None, None]
        vtr = [None, None]
        lg_mm = [None, None]
        v_all = []
        h_all = []
        ex_rd = [None, None]
        c3 = {}
        wrv = wr_s[:, :].rearrange("p (k e) -> p k e", e=16)
        x2t = {}

        def x2_load(t):
            b = t % 2
            x2t[t] = B.dma("sp", xb2[b], x[t * 128:(t + 1) * 128, :], x2s[b], [xrd[b], s3b_mm])

        def mm_group(t, nb):
            b = t % 2
            hf, tl = t // 8, t % 8
            bk = nb
            if nb == 0:
                if t == 8:
                    B.wait("pool", [c3.get("mm")])
                    conv_emit(6)
            for j in range(16):
                f = lambda e, bk=bk, j=j, hf=hf, tl=tl, nb=nb: e.matmul(
                    ps[bk][:, :], mg[hf][:, j, tl * 128:(tl + 1) * 128], woA[:, j, nb * 512:(nb + 1) * 512],
                    start=(j == 0), stop=(j == 15))
                if j < 15:
                    B.op0("pe", f, [wo_toks[nb], mg_toks, psfree[bk]])
                else:
                    mt = B.op("pe", f)
            ad = B.op("dve", lambda e, bk=bk, nb=nb, b=b: e.tensor_tensor(
                hb[b][:, nb * 512:(nb + 1) * 512], ps[bk][:, :], xb2[b][:, nb * 512:(nb + 1) * 512], ALU.add),
                [mt, x2t[t], h_st[b], s3b_mm])
            psfree[bk] = ad
            c3["mm"] = mt
            if nb == 0:
                c3["adds"] = []
            c3["adds"].append(ad)
            if nb == 3:
                xrd[b] = ad
                if t + 1 < 16:
                    x2_load(t + 1)

        def norm_v(t):
            b = t % 2
            adds = c3["adds"]
            sq = B.op("act", lambda e, b=b, t=t: e.activation(vb[b], hb[b], AF.Square, accum_out=ssq2[:, t:t + 1]),
                      [adds, v_st[b], vtr[b], s3b_mm])
            r1 = B.op("act", lambda e, t=t: e.activation(rstd2[:, t:t + 1], ssq2[:, t:t + 1], AF.Sqrt, bias=EPS,
                                                        scale=1.0 / D), [sq])
            r2 = B.op("dve", lambda e, t=t: e.reciprocal(rstd2[:, t:t + 1], rstd2[:, t:t + 1]), [r1])
            vv = B.op("dve", lambda e, b=b, t=t: e.scalar_tensor_tensor(
                vb[b], hb[b], rstd2[:, t:t + 1], gmoe_s, ALU.mult, ALU.mult), [r2, gmoe_tok, sq])
            c3["vv%d" % t] = vv
            c3["vv"] = vv
            h_st[b] = B.dma("sp", hbuf[t * 128:(t + 1) * 128, :], hb[b], hst[b], [adds, sq])
            v_st[b] = B.dma("sp", vbuf[t * 128:(t + 1) * 128, :], vb[b], vst[b], [vv])
            h_all.append(h_st[b])
            v_all.append(v_st[b])

        def st_tr(t):
            b = t % 2
            vv = c3["vv%d" % t]
            for k in range(16):
                bk = 4 + k // 8
                f = lambda e, bk=bk, k=k, b=b: e.transpose(
                    psb(bk)[:, (k % 8) * 128:(k % 8 + 1) * 128], vb[b][:, k * 128:(k + 1) * 128], identb[:, :])
                if k < 15:
                    B.op0("pe", f, [vv, psfree[4], psfree[5]])
                else:
                    tr = B.op("pe", f)
            vtr[b] = tr
            e1 = B.op("act", lambda e, b=b: e.activation(
                vT[b][:, 0:8, :], psb(4).rearrange("p (k c) -> p k c", k=8), AF.Copy), [tr, lg_mm[b]])
            e2 = B.op("act", lambda e, b=b: e.activation(
                vT[b][:, 8:16, :], psb(5).rearrange("p (k c) -> p k c", k=8), AF.Copy), [tr])
            psfree[4] = e1
            psfree[5] = e2
            c3["e12"] = [e1, e2]

        def st_lg(t):
            b = t % 2
            for k in range(16):
                f = lambda e, k=k, b=b: e.matmul(ps[6][:, 0:16], vT[b][:, k, :], wrv[:, k, :],
                                                 start=(k == 0), stop=(k == 15))
                if k < 15:
                    B.op0("pe", f, c3["e12"] + [wr_tok, psfree[6]])
                else:
                    lm = B.op("pe", f)
            lg_mm[b] = lm
            c3["lm"] = lm
            m1 = B.op("dve", lambda e, t=t: e.reduce_max(smx[:, t:t + 1], ps[6][:, 0:16], AX.X), [lm])
            m2 = B.op("dve", lambda e, t=t: e.tensor_scalar(smx[:, t:t + 1], smx[:, t:t + 1], -1.0, None, ALU.mult),
                      [m1])
            ex = B.op("act", lambda e, t=t, b=b: e.activation(
                exb[:, b, :], ps[6][:, 0:16], AF.Exp, bias=smx[:, t:t + 1], accum_out=ssm[:, t:t + 1]),
                [m2, ex_rd[b]])
            psfree[6] = ex
            r3 = B.op("dve", lambda e, t=t: e.reciprocal(ssm[:, t:t + 1], ssm[:, t:t + 1]), [ex])
            af = B.op("dve", lambda e, t=t, b=b: e.tensor_scalar(
                afv[:, t, :], exb[:, b, :], ssm[:, t:t + 1], None, ALU.mult), [r3])
            ex_rd[b] = af
            c3["af"] = af

        def st_at(t):
            tq = t % 4
            at = B.op("pe", lambda e, t=t, tq=tq: e.transpose(
                ps[7][0:16, tq * 128:(tq + 1) * 128], afv[:, t, :], identf[:, :]),
                [c3["af"], psfree[7] if tq == 0 else None])
            if tq == 3:
                q4 = t // 4
                c3["aff_ev"] = B.op("act", lambda e, q4=q4: e.activation(
                    affT[0:16, q4 * 512:(q4 + 1) * 512], ps[7][0:16, :], AF.Copy), [at, s3b_mm])
                psfree[7] = c3["aff_ev"]

        x2_load(0)
        for t in range(17):
            if t < 16:
                mm_group(t, 0)
            if t >= 2:
                st_at(t - 2)
            if t < 16:
                mm_group(t, 1)
            if t >= 1:
                st_tr(t - 1)
            if t < 16:
                mm_group(t, 2)
            if t >= 1:
                st_lg(t - 1)
            if t < 16:
                mm_group(t, 3)
                norm_v(t)
        st_at(15)
        s3c_mm = c3["mm"]
        adds = c3["adds"]
        vv = c3["vv"]
        lm = c3["lm"]
        aff_ev = c3["aff_ev"]

        r2_slots = [bfv(rA[0] + i * 8 * K, 8 * K) for i in range(12)]
        ch2 = []
        for e_ in range(NE):
            ci = e_ - (NE - NCONV)
            for ft in range(NFT):
                ch2.append((egu[e_, ft] if ci < 0 else egu_bf[ci, ft], 4096))
            for nq in range(8):
                ch2.append((edn[e_, nq] if ci < 0 else edn_bf[ci, nq], 2816))
        R2 = Stream(B, r2_slots, ch2, start_waits=[s3c_mm],
                    extra_waits={(NE - NCONV) * 19: [conv_done]}, hwdge_from=(NE - NCONV) * 19,
                    after_issue=lambda j_: conv_emit(1000 if j_ == 11 else 0))
        R2.prefetch()

        gfin_s = f32v(rD[0], 2048)
        gfs = B.dsem("gfs")
        gfin_tok = B.dma("sp", gfin_s, gfin_d, gfs, [vv])
        work = f32v(rB[0], 2048)
        vals = f32v(rB[0] + 8 * K, 256)
        idxs = arena[:, rB[0] + 9 * K: rB[0] + 9 * K + 512].bitcast(U32)
        idxf = f32v(rB[0] + 10 * K, 256)
        w0 = B.op("dve", lambda e: e.tensor_copy(work[0:16, :], affT[0:16, :]), [aff_ev, adds, xrd[0], xrd[1]])
        prev = w0
        for it in range(32):
            sl = slice(it * 8, it * 8 + 8)
            a = B.op("dve", lambda e, sl=sl: e.max(vals[0:16, sl], work[0:16, :]), [prev])
            bq = B.op("dve", lambda e, sl=sl: e.max_index(idxs[0:16, sl], vals[0:16, sl], work[0:16, :]), [a])
            prev = B.op("dve", lambda e, sl=sl: e.match_replace(work[0:16, :], vals[0:16, sl], work[0:16, :], -1.0),
                        [bq])
        cf = B.op("dve", lambda e: e.tensor_copy(idxf[0:16, :], idxs[0:16, :]), [prev])
        for ct in range(2):
            B.op0("pe", lambda e, ct=ct: e.transpose(
                ps[0][:, ct * 16:(ct + 1) * 16], idxf[0:16, ct * 128:(ct + 1) * 128], identf[0:16, 0:16]),
                [cf, psfree[0]])
            trk = B.op("pe", lambda e, ct=ct: e.transpose(
                ps[0][:, 32 + ct * 16:32 + (ct + 1) * 16], vals[0:16, ct * 128:(ct + 1) * 128], identf[0:16, 0:16]))
        i1 = B.op("dve", lambda e: e.tensor_copy(idxT[:, :], ps[0][:, 0:32]), [trk])
        i2 = B.op("dve", lambda e: e.tensor_copy(gateT[:, :], ps[0][:, 32:64]), [trk, i1])
        psfree[0] = i2
        route_tok = i2
        idxTv = idxT[:, :].rearrange("p (c e) -> p c e", e=16)
        gateTv = gateT[:, :].rearrange("p (c e) -> p c e", e=16)

        xs = [bfv(rB[0] + 16 * K, 4096).rearrange("p (c d) -> p c d", c=2),
              bfv(rB[0] + 24 * K, 4096).rearrange("p (c d) -> p c d", c=2)]
        xsT = [bfv(rB[0] + 32 * K, 4096).rearrange("p (k c) -> p k c", k=16),
               bfv(rB[0] + 40 * K, 4096).rearrange("p (k c) -> p k c", k=16)]
        h1T = [bfv(rB[0] + 48 * K, 2816).rearrange("p (f c) -> p f c", f=11),
               bfv(rB[0] + 54 * K, 2816).rearrange("p (f c) -> p f c", f=11)]
        ybuf = [f32v(rD[0] + 8 * K, 2048), f32v(rD[0] + 16 * K, 2048)]
        silt = [f32v(rB[0] + 60 * K, 256), f32v(rB[0] + 61 * K, 256)]
        gsem = [B.dsem(), B.dsem()]
        scs = B.dsem("scs")
        g_tok = [None, None]
        xs_rd = [None, None]
        xsT_rd = [None, None]
        h1_rd = [None, None]
        sc_tok = None
        y_rd = [None, None]
        sil_rd = [None, None]
        moe_pre = [route_tok, cf, prev] + v_all + h_all + [aff_ev]

        def gather(e_):
            b = e_ % 2
            tk = None
            for ct in range(2):
                tk = B.raw("pool", lambda e, ct=ct, b=b, e_=e_: e.indirect_dma_start(
                    out=xs[b][:, ct, :], out_offset=None, in_=vbuf[:, :],
                    in_offset=bass.IndirectOffsetOnAxis(ap=idxTv[:, ct, e_:e_ + 1], axis=0)),
                    gsem[b], moe_pre + [xs_rd[b], lm, s3c_mm])
            g_tok[b] = tk

        gather(0)
        evn = 0
        for e_ in range(NE):
            b = e_ % 2
            if e_ + 1 < NE:
                gather(e_ + 1)
            for q in range(4):
                bk = q % 2
                for kk in range(4):
                    k = q * 4 + kk
                    for ct in range(2):
                        f = lambda e, bk=bk, kk=kk, ct=ct, k=k, b=b: e.transpose(
                            psb(bk)[:, kk * 256 + ct * 128: kk * 256 + (ct + 1) * 128],
                            xs[b][:, ct, k * 128:(k + 1) * 128], identb[:, :])
                        if kk == 3 and ct == 1:
                            tr = B.op("pe", f)
                        else:
                            B.op0("pe", f, [g_tok[b], psfree[bk], i2])
                w = [tr, xsT_rd[b]]
                if q % 2 == 0:
                    ev = B.op("act", lambda e, bk=bk, q=q, b=b: e.activation(
                        xsT[b][:, q * 4:(q + 1) * 4, :], psb(bk).rearrange("p (k c) -> p k c", k=4), AF.Copy), w)
                else:
                    ev = B.op("dve", lambda e, bk=bk, q=q, b=b: e.tensor_copy(
                        xsT[b][:, q * 4:(q + 1) * 4, :], psb(bk).rearrange("p (k c) -> p k c", k=4)), w)
                psfree[bk] = ev
                if q == 2:
                    xt_a = ev
                if q == 3:
                    xt_d = ev
            xs_rd[b] = tr
            xsT_w = [xt_a, xt_d]
            hl = {}
            for ft in range(NFT):
                cv, ctk = R2.get()
                cv = cv.rearrange("p (g k c) -> p g k c", g=2, k=16)
                st = ft % 2
                bg, bu = 2 + 2 * st, 3 + 2 * st
                for gu in range(2):
                    bk = bg if gu == 0 else bu
                    for k in range(16):
                        f = lambda e, bk=bk, gu=gu, k=k, cv=cv, b=b: e.matmul(
                            ps[bk][:, 0:256], cv[:, gu, k, :], xsT[b][:, k, :], start=(k == 0), stop=(k == 15))
                        if k < 15:
                            B.op0("pe", f, [ctk, xsT_w, psfree[bk]])
                        else:
                            mt = B.op("pe", f)
                    if gu == 0:
                        mg_ = mt
                R2.release(mt)
                sl_ = silt[st]
                a1 = B.op("act", lambda e, sl_=sl_, bg=bg: e.activation(sl_, ps[bg][:, 0:256], AF.Silu),
                          [mg_, sil_rd[st]])
                d1 = B.op("dve", lambda e, sl_=sl_, bu=bu, ft=ft, b=b: e.tensor_tensor(
                    h1T[b][:, ft, :], sl_, ps[bu][:, 0:256], ALU.mult), [a1, mt, h1_rd[b]])
                psfree[bg] = a1
                psfree[bu] = d1
                sil_rd[st] = d1
                hl[ft] = d1
            xsT_rd[b] = mt
            ylast = [[], []]
            for nq in range(8):
                cv, ctk = R2.get()
                cv = cv.rearrange("p (f n) -> p f n", f=11)
                for ct in range(2):
                    bk = 6 + ct
                    for ft in range(NFT):
                        f = lambda e, bk=bk, ft=ft, ct=ct, cv=cv, b=b: e.matmul(
                            ps[bk][:, 0:256], h1T[b][:, ft, ct * 128:(ct + 1) * 128], cv[:, ft, :],
                            start=(ft == 0), stop=(ft == NFT - 1))
                        if ft < NFT - 1:
                            B.op0("pe", f, [ctk, hl[NFT - 1], hl[NFT - 2], psfree[bk]])
                        else:
                            mt = B.op("pe", f)
                    w = [mt, y_rd[ct], route_tok] + (v_all if e_ == 0 else [])
                    if ct == 0:
                        ev = B.op("act", lambda e, bk=bk, ct=ct, nq=nq, e_=e_: e.activation(
                            ybuf[ct][:, nq * 256:(nq + 1) * 256], ps[bk][:, 0:256], AF.Copy,
                            scale=gateTv[:, ct, e_:e_ + 1]), w)
                    else:
                        ev = B.op("dve", lambda e, bk=bk, ct=ct, nq=nq, e_=e_: e.tensor_scalar(
                            ybuf[ct][:, nq * 256:(nq + 1) * 256], ps[bk][:, 0:256], gateTv[:, ct, e_:e_ + 1],
                            None, ALU.mult), w)
                    psfree[bk] = ev
                    ylast[ct] = ev
                R2.release(mt)
            h1_rd[b] = mt
            moe_mm = mt
            for ct in range(2):
                sc_tok = B.raw("pool", lambda e, ct=ct, e_=e_: e.indirect_dma_start(
                    out=hbuf[:, :], out_offset=bass.IndirectOffsetOnAxis(ap=idxTv[:, ct, e_:e_ + 1], axis=0),
                    in_=ybuf[ct][:, :], in_offset=None, compute_op=ALU.add),
                    scs, [ylast[ct], sc_tok] + h_all)
                y_rd[ct] = sc_tok

        NB6 = 4
        fin = [f32v(rA[0] + i * 8 * K, 2048) for i in range(NB6)]
        fo = [f32v(rA[0] + (NB6 + i) * 8 * K, 2048) for i in range(NB6)]
        fjunk = bfv(rA[0] + 2 * NB6 * 8 * K, 2048)
        fls = [B.dsem() for _ in range(NB6)]
        fss = [B.dsem() for _ in range(NB6)]
        ssq3 = stat[:, 0:16]
        rstd3 = stat[:, 16:32]
        f_rd = [None] * NB6
        o_st = [None] * NB6
        o_all = []
        sqp = None
        lts = {}

        def f_load(t):
            b = t % NB6
            lts[t] = B.dma("sp", fin[b], hbuf[t * 128:(t + 1) * 128, :], fls[b], [sc_tok, moe_mm, f_rd[b]])

        for t in range(NB6):
            f_load(t)
        for t in range(16):
            b = t % NB6
            lt = lts[t]
            sq = B.op("act", lambda e, b=b, t=t: e.activation(fjunk, fin[b], AF.Square, accum_out=ssq3[:, t:t + 1]),
                      [lt, moe_mm, sqp])
            sqp = sq
            r1 = B.op("act", lambda e, t=t: e.activation(rstd3[:, t:t + 1], ssq3[:, t:t + 1], AF.Sqrt, bias=EPS,
                                                        scale=1.0 / D), [sq])
            r2 = B.op("dve", lambda e, t=t: e.reciprocal(rstd3[:, t:t + 1], rstd3[:, t:t + 1]), [r1])
            oo = B.op("dve", lambda e, b=b, t=t: e.scalar_tensor_tensor(
                fo[b], fin[b], rstd3[:, t:t + 1], gfin_s, ALU.mult, ALU.mult), [r2, gfin_tok, o_st[b], moe_mm])
            f_rd[b] = oo
            if t + NB6 < 16:
                f_load(t + NB6)
            o_st[b] = B.dma("sp", out[t * 128:(t + 1) * 128, :], fo[b], fss[b], [oo])
            o_all.append(o_st[b])
        B.wait("sp", o_all[-NB6:])
        if debug:
            dbs = B.dsem("dbs")
            B.wait("sp", [i2])
            t1 = B.dma("sp", dbg[:, 0:256], afftm[:, :], dbs)
            t2 = B.dma("sp", dbg[:, 256:288], gateT[:, :], dbs)
            t3 = B.dma("sp", dbg[:, 288:320], idxT[:, :].bitcast(F32), dbs)
            B.wait("sp", [t3])

        with nc.Block() as block:
            @block.tensor
            def _(e):
                for f in B.prog["pe"]:
                    f(e)

            @block.scalar
            def _(e):
                for f in B.prog["act"]:
                    f(e)

            @block.vector
            def _(e):
                for f in B.prog["dve"]:
                    f(e)

            @block.gpsimd
            def _(e):
                for f in B.prog["pool"]:
                    f(e)

            @block.sync
            def _(e):
                for f in B.prog["sp"]:
                    f(e)
    return nc


_CONST = {}


def _constants():
    if _CONST:
        return _CONST
    bf = ml_dtypes.bfloat16
    n = np.arange(S, dtype=np.float64)
    ang = 2.0 * np.pi * ((n[:, None] * n[None, :]) % S) / S
    dft = np.empty((2, 2, 128, 8192), dtype=bf)
    for c_s, m in enumerate((np.cos(ang), np.sin(ang))):
        mb = m.astype(np.float32).astype(bf)
        for kb in range(2):
            blk = mb[:, kb * 512:(kb + 1) * 512].reshape(16, 128, 512).transpose(1, 0, 2)
            dft[kb, c_s] = blk.reshape(128, 8192)
    c = np.arange(256, dtype=np.float64)
    ang2 = 2.0 * np.pi * ((c[:, None] * c[None, :]) % 256) / 256
    ccsc = np.empty((128, 2, 2, 256), dtype=bf)
    for c_s, m in enumerate((np.cos(ang2), np.sin(ang2))):
        ccsc[:, c_s] = m.astype(np.float32).astype(bf).reshape(2, 128, 256).transpose(1, 0, 2)
    poolr = np.zeros((4, 2, 128, 2, 6, 512), dtype=np.float32)
    i = np.arange(S)
    for g, w in enumerate((2, 4, 8, 16)):
        half = w // 2
        lo = np.clip(i - half, 0, S)
        hi = np.clip(i + half, 0, S)
        P = np.zeros((S, S), dtype=np.float64)
        for ii in range(S):
            P[ii, lo[ii]:hi[ii]] = 1.0 / (hi[ii] - lo[ii])
            P[ii, ii] -= 1.0
        PT = P.T
        for ib in range(4):
            for jj in range(6):
                jt = 4 * ib - 1 + jj
                if 0 <= jt < 16:
                    poolr[ib, g // 2, :, g % 2, jj, :] = PT[jt * 128:(jt + 1) * 128, ib * 512:(ib + 1) * 512]
    _CONST["dft"] = dft
    _CONST["ccsc"] = ccsc.reshape(128, 1024)
    _CONST["poolr"] = poolr.astype(bf).reshape(4, 2, 128, 6144)
    _CONST["alt"] = np.tile(np.where(np.arange(128) % 2 == 0, 1.0, -1.0).astype(np.float32)[:, None], (1, 16)).astype(bf)
    _CONST["identb"] = np.eye(128, dtype=np.float32).astype(bf)
    _CONST["identf"] = np.eye(128, dtype=np.float32)
    return _CONST


def _layout_weights(inp):
    f = lambda a: np.ascontiguousarray(np.asarray(a, dtype=np.float32))
    W = {}
    w_in = f(inp["w_in"])[0]
    W["win"] = w_in.reshape(16, 128, 4, 512).transpose(2, 1, 0, 3).reshape(4, 128, 8192).copy()
    wg = f(inp["w_gate"])[0].reshape(16, 128, 2, 16, 128)
    wbf = f(inp["w_branch_f"])[0].reshape(8, 128, 16, 128)
    wbp = f(inp["w_branch_p"])[0].reshape(8, 128, 16, 128)
    wmix = np.empty((16, 128, 6144), dtype=np.float32)
    wmix[:, :, 0:4096] = wg.transpose(3, 1, 2, 0, 4).reshape(16, 128, 4096)
    wmix[:, :, 4096:5120] = wbf.transpose(2, 1, 0, 3).reshape(16, 128, 1024)
    wmix[:, :, 5120:6144] = wbp.transpose(2, 1, 0, 3).reshape(16, 128, 1024)
    W["wmix"] = wmix
    wo = f(inp["w_out"])[0]
    W["wout"] = wo.reshape(16, 128, 4, 512).transpose(2, 1, 0, 3).reshape(4, 128, 8192).copy()
    W["wfm"] = f(inp["w_fourier_mix"])[0].reshape(4, 2, 128, 256).transpose(2, 0, 1, 3).reshape(128, 2048).copy()
    W["wpm"] = f(inp["w_pool_mix"])[0].reshape(4, 2, 128, 256).transpose(2, 0, 1, 3).reshape(128, 2048).copy()
    eg = f(inp["w_expert_gate"])[0].reshape(NE, 16, 128, NFT, 128)
    eu = f(inp["w_expert_up"])[0].reshape(NE, 16, 128, NFT, 128)
    egu = np.empty((NE, NFT, 128, 2, 16, 128), dtype=np.float32)
    egu[:, :, :, 0] = eg.transpose(0, 3, 2, 1, 4)
    egu[:, :, :, 1] = eu.transpose(0, 3, 2, 1, 4)
    W["egu"] = egu.reshape(NE, NFT, 128, 4096)
    ed = f(inp["w_expert_down"])[0].reshape(NE, NFT, 128, 8, 256)
    W["edn"] = ed.transpose(0, 3, 2, 1, 4).reshape(NE, 8, 128, 2816).copy()
    W["gmix"] = f(inp["norm_mix_g"])[0].reshape(16, 128).T.copy()
    W["bgate"] = f(inp["b_gate"])[0].reshape(32, 128).T.copy()
    W["pscale"] = f(inp["pool_scale"])[0].reshape(8, 128).T.copy()
    W["gmoe"] = np.broadcast_to(f(inp["norm_moe_g"])[0][None, :], (128, D)).copy()
    W["gfin"] = np.broadcast_to(f(inp["norm_final_g"])[None, :], (128, D)).copy()
    W["wr"] = f(inp["w_router"])[0].reshape(16, 128, 16).transpose(1, 0, 2).reshape(128, 256).copy()
    W.update(_constants())
    return W


_NC = {}


def kernel(**inputs):
    W = _layout_weights(inputs)
    xin = np.ascontiguousarray(np.asarray(inputs["x"], dtype=np.float32))
    if "nc" not in _NC:
        _NC["nc"] = build_nc()
    nc = _NC["nc"]
    in_maps = []
    for b in range(8):
        m = dict(W)
        m["x"] = xin[b]
        in_maps.append(m)
    res = run_bass_kernel_spmd(nc, in_maps, core_ids=list(range(8)))
    return np.stack([np.asarray(r["out"], dtype=np.float32) for r in res.results], axis=0)
```
